# Optimizing a Trainium2 kernel written in Bass

```python
import jax
import jax.numpy as jnp
from jax import lax
import numpy as np

D_MODEL = 2048
BATCH = 2
SEQ = 4096
DEPTH = 4

GRID_W = 64
CTX_LEN = 256
CHUNK = 128
EPS = 1e-6
CONV_W = 4
DIRECTIONS = (False, True)

RET_HEADS = 8
RET_DV = D_MODEL // RET_HEADS
RET_DK = RET_DV // 2
RET_QK = RET_HEADS * RET_DK
RET_V = RET_HEADS * RET_DV
ROPE_BASE = 10000.0

SSD_INNER = D_MODEL
SSD_HEADDIM = 64
SSD_HEADS = SSD_INNER // SSD_HEADDIM
SSD_GROUPS = 4
SSD_STATE = 128
SSD_CONV_DIM = SSD_INNER + 2 * SSD_GROUPS * SSD_STATE

LRU_WIDTH = D_MODEL
LRU_BLOCKS = 16
LRU_BW = LRU_WIDTH // LRU_BLOCKS
LRU_C = 8.0

N_BRANCH = 3
BRANCH_W = D_MODEL
IN_SPLITS = (RET_QK, RET_QK, RET_V, RET_V, SSD_INNER, SSD_CONV_DIM, 2 * SSD_HEADS,
             LRU_WIDTH, LRU_WIDTH, N_BRANCH * D_MODEL)
D_IN = sum(IN_SPLITS)

D_FF = 5632
N_EXPERTS = 8
TOP_K = 2
D_FF_EXPERT = 2 * D_MODEL

kernel_name = "hybrid_ret_ssd_rglru_moe_dit"

F32 = jnp.float32


def rmsnorm(x, g):
    xf = x.astype(F32)
    y = xf * lax.rsqrt(jnp.mean(xf * xf, axis=-1, keepdims=True) + EPS)
    return (y * g.astype(F32)).astype(x.dtype)


def split_in(p):
    offs, acc = [], 0
    for s in IN_SPLITS[:-1]:
        acc += s
        offs.append(acc)
    return jnp.split(p, offs, axis=-1)


def dwconv(x, w, b):
    y = lax.conv_general_dilated(
        x, w[:, None, :].astype(x.dtype), window_strides=(1,),
        padding=[(CONV_W // 2, CONV_W - 1 - CONV_W // 2)],
        dimension_numbers=('NWC', 'WIO', 'NWC'), feature_group_count=x.shape[-1])
    return y + b.astype(x.dtype)


def to_heads(t, d):
    return t.astype(F32).reshape(t.shape[0], t.shape[1], -1, d)


def axial_rotary(n_lat):
    rows = n_lat // GRID_W
    row = jnp.repeat(jnp.arange(rows, dtype=F32), GRID_W)
    col = jnp.tile(jnp.arange(GRID_W, dtype=F32), rows)
    n_freq = RET_DK // 4
    inv = ROPE_BASE ** (-jnp.arange(n_freq, dtype=F32) / n_freq)
    ang = jnp.concatenate([row[:, None] * inv, col[:, None] * inv], axis=-1)[:, None, :]
    return jnp.cos(ang), jnp.sin(ang)


def apply_rot(t, cos, sin):
    t1, t2 = t[..., :RET_DK // 2], t[..., RET_DK // 2:]
    return jnp.concatenate([t1 * cos - t2 * sin, t1 * sin + t2 * cos], axis=-1)


def scan_ctx_then_latent(scan_fn, ctx_seq, lat_seq, consts, s0, reverse):
    order = (lambda t: jnp.flip(t, axis=1)) if reverse else (lambda t: t)
    y_ctx, s_ctx = scan_fn(*[order(t) for t in ctx_seq], *consts, s0)
    y_lat, _ = scan_fn(*[order(t) for t in lat_seq], *consts, s_ctx)
    return order(y_ctx), order(y_lat)


def retention_chunked(q, k, v, log_g, s0):
    b, n, h, dk = q.shape
    dv = v.shape[-1]
    nc = n // CHUNK
    qc = q.reshape(b, nc, CHUNK, h, dk)
    kc = k.reshape(b, nc, CHUNK, h, dk)
    vc = v.reshape(b, nc, CHUNK, h, dv)
    idx = jnp.arange(CHUNK, dtype=F32)
    diff = idx[:, None] - idx[None, :]
    decay = jnp.where(diff >= 0, jnp.exp(log_g[:, None, None] * jnp.maximum(diff, 0.0)), 0.0)
    scores = jnp.einsum('bcihd,bcjhd->bchij', qc, kc) * decay
    y_intra = jnp.einsum('bchij,bcjhe->bcihe', scores, vc)
    k_decay = jnp.exp(log_g[None, :] * (CHUNK - 1 - idx)[:, None])
    kv = jnp.einsum('bcjhd,jh,bcjhe->bchde', kc, k_decay, vc)
    chunk_decay = jnp.exp(log_g * CHUNK)[None, :, None, None]

    def step(s, kv_c):
        return chunk_decay * s + kv_c, s

    s_final, s_in = lax.scan(step, s0, jnp.moveaxis(kv, 1, 0))
    s_in = jnp.moveaxis(s_in, 0, 1)
    q_decay = jnp.exp(log_g[None, :] * (idx + 1.0)[:, None])
    y_cross = jnp.einsum('bcihd,ih,bchde->bcihe', qc, q_decay, s_in)
    return (y_intra + y_cross).reshape(b, n, h, dv), s_final


def ssd_chunked(x, a, bm, cm, s0):
    b, n, h, p = x.shape
    g, ns = bm.shape[2], bm.shape[3]
    hg = h // g
    nc = n // CHUNK
    xc = x.reshape(b, nc, CHUNK, g, hg, p)
    ac = a.reshape(b, nc, CHUNK, g, hg)
    bc = bm.reshape(b, nc, CHUNK, g, ns)
    cc = cm.reshape(b, nc, CHUNK, g, ns)
    a_cum = jnp.cumsum(ac, axis=2)
    seg = a_cum[:, :, :, None] - a_cum[:, :, None]
    causal = jnp.tril(jnp.ones((CHUNK, CHUNK), dtype=bool))[:, :, None, None]
    lmask = jnp.exp(jnp.where(causal, seg, -jnp.inf))
    cb = jnp.einsum('bclgn,bcsgn->bclsg', cc, bc)
    y_diag = jnp.einsum('bclsg,bclsgh,bcsghp->bclghp', cb, lmask, xc)
    decay_to_end = jnp.exp(a_cum[:, :, -1:] - a_cum)
    states = jnp.einsum('bclgn,bclgh,bclghp->bcghpn', bc, decay_to_end, xc)
    chunk_decay = jnp.exp(a_cum[:, :, -1])

    def step(s, inp):
        st, dec = inp
        return dec[..., None, None] * s + st, s

    s_fin, s_in = lax.scan(step, s0.reshape(b, g, hg, p, ns),
                           (jnp.moveaxis(states, 1, 0), jnp.moveaxis(chunk_decay, 1, 0)))
    s_in = jnp.moveaxis(s_in, 0, 1)
    y_off = jnp.einsum('bclgn,bcghpn,bclgh->bclghp', cc, s_in, jnp.exp(a_cum))
    return (y_diag + y_off).reshape(b, n, h, p), s_fin.reshape(b, h, p, ns)


def linear_scan(a, bx, h0):
    bx = bx.at[:, 0].add(a[:, 0] * h0)

    def combine(l, r):
        al, bl = l
        ar, br = r
        return al * ar, ar * bl + br

    _, h = lax.associative_scan(combine, (a, bx), axis=1)
    return h, h[:, -1]


def ssd_prepare(xbc, dt_raw, conv_w, conv_b, dt_bias):
    b, n, _ = xbc.shape
    xbc = jax.nn.silu(dwconv(xbc, conv_w, conv_b).astype(F32))
    xs, bm, cm = jnp.split(xbc, [SSD_INNER, SSD_INNER + SSD_GROUPS * SSD_STATE], axis=-1)
    dt = jax.nn.softplus(dt_raw.astype(F32).reshape(b, n, 2, SSD_HEADS) + dt_bias.astype(F32))
    return (xs.reshape(b, n, SSD_HEADS, SSD_HEADDIM), bm.reshape(b, n, SSD_GROUPS, SSD_STATE),
            cm.reshape(b, n, SSD_GROUPS, SSD_STATE), dt)


def lru_gates(xr, w_gates, b_gates, lam):
    b, n, _ = xr.shape
    xb = xr.reshape(b, n, LRU_BLOCKS, LRU_BW)
    gates = jnp.einsum('bnkd,gkde->gbnke', xb, w_gates.astype(F32)).reshape(2, b, n, LRU_WIDTH)
    gates = jax.nn.sigmoid(gates + b_gates.astype(F32)[:, None, None, :])
    r, i = gates[0], gates[1]
    log_a = -LRU_C * r * jax.nn.softplus(-lam.astype(F32))
    a = jnp.exp(log_a)
    mult = jnp.sqrt(jnp.maximum(-jnp.expm1(2.0 * log_a), 0.0))
    return a, mult * (i * xr)


def head_groupnorm(y, g):
    b, n = y.shape[0], y.shape[1]
    mu = jnp.mean(y, axis=-1, keepdims=True)
    var = jnp.mean(jnp.square(y - mu), axis=-1, keepdims=True)
    return ((y - mu) * lax.rsqrt(var + EPS)).reshape(b, n, -1) * g.astype(F32)


def branch_merge(u_ret, u_ssd, u_lru, merge_raw, w_branch, w_out):
    u = jnp.stack([u_ret, u_ssd, u_lru], axis=2)
    p = jnp.einsum('bnkw,kwd->bnkd', u, w_branch)
    gates = jax.nn.sigmoid(merge_raw.astype(F32)).astype(p.dtype).reshape(p.shape)
    return jnp.sum(gates * p, axis=2) @ w_out


def mixer_forward(hl, hc, cos_l, sin_l, w_in, ret_decay, ret_gn_g, ssd_conv_w, ssd_conv_b,
                  ssd_dt_bias, ssd_a_log, ssd_d, ssd_norm_g, lru_conv_w, lru_conv_b,
                  lru_gate_w, lru_gate_b, lru_lambda, w_branch, w_out, ctx_out):
    dtype = hl.dtype
    b = hl.shape[0]
    ql, kl, vl, gl, zl, xbcl, dtl, gatel, recl, mergel = split_in(hl @ w_in)
    qc, kc, vc, gc, zc, xbcc, dtc, gatec, recc, mergec = split_in(hc @ w_in)

    scale = RET_DK ** -0.5
    lat_seq = (apply_rot(to_heads(ql, RET_DK), cos_l, sin_l),
               apply_rot(to_heads(kl, RET_DK), cos_l, sin_l) * scale,
               to_heads(vl, RET_DV))
    ctx_seq = (to_heads(qc, RET_DK), to_heads(kc, RET_DK) * scale, to_heads(vc, RET_DV))
    s0 = jnp.zeros((b, RET_HEADS, RET_DK, RET_DV), F32)
    ret_l, ret_c = 0.0, 0.0
    for d, rev in enumerate(DIRECTIONS):
        log_g = jax.nn.log_sigmoid(ret_decay[d].astype(F32))
        yc, yl = scan_ctx_then_latent(retention_chunked, ctx_seq, lat_seq, (log_g,), s0, rev)
        ret_l, ret_c = ret_l + yl, ret_c + yc

    A = -jnp.exp(ssd_a_log.astype(F32))
    xs_l, b_l, c_l, dt_l = ssd_prepare(xbcl, dtl, ssd_conv_w, ssd_conv_b, ssd_dt_bias)
    xs_c, b_c, c_c, dt_c = ssd_prepare(xbcc, dtc, ssd_conv_w, ssd_conv_b, ssd_dt_bias)
    s0 = jnp.zeros((b, SSD_HEADS, SSD_HEADDIM, SSD_STATE), F32)
    d_skip = ssd_d.astype(F32)[:, None]
    ssd_l, ssd_c = d_skip * xs_l, d_skip * xs_c
    for d, rev in enumerate(DIRECTIONS):
        seq_c = (xs_c * dt_c[:, :, d, :, None], dt_c[:, :, d] * A[d], b_c, c_c)
        seq_l = (xs_l * dt_l[:, :, d, :, None], dt_l[:, :, d] * A[d], b_l, c_l)
        yc, yl = scan_ctx_then_latent(ssd_chunked, seq_c, seq_l, (), s0, rev)
        ssd_l, ssd_c = ssd_l + yl, ssd_c + yc

    xr_l = dwconv(recl, lru_conv_w, lru_conv_b).astype(F32)
    xr_c = dwconv(recc, lru_conv_w, lru_conv_b).astype(F32)
    h0 = jnp.zeros((b, LRU_WIDTH), F32)
    lru_l, lru_c = 0.0, 0.0
    for d, rev in enumerate(DIRECTIONS):
        a_l, bx_l = lru_gates(xr_l, lru_gate_w[d], lru_gate_b[d], lru_lambda[d])
        a_c, bx_c = lru_gates(xr_c, lru_gate_w[d], lru_gate_b[d], lru_lambda[d])
        yc, yl = scan_ctx_then_latent(linear_scan, (a_c, bx_c), (a_l, bx_l), (), h0, rev)
        lru_l, lru_c = lru_l + yl, lru_c + yc

    def branch_outputs(ret, g, ssd, z, lru, gate):
        n = ret.shape[1]
        u_ret = (jax.nn.silu(g.astype(F32)) * head_groupnorm(ret, ret_gn_g)).astype(dtype)
        u_ssd = rmsnorm(ssd.reshape(b, n, SSD_INNER) * jax.nn.silu(z.astype(F32)), ssd_norm_g).astype(dtype)
        u_lru = (lru * jax.nn.gelu(gate.astype(F32))).astype(dtype)
        return u_ret, u_ssd, u_lru

    out_l = branch_merge(*branch_outputs(ret_l, gl, ssd_l, zl, lru_l, gatel), mergel, w_branch, w_out)
    out_c = None
    if ctx_out:
        out_c = branch_merge(*branch_outputs(ret_c, gc, ssd_c, zc, lru_c, gatec), mergec, w_branch, w_out)
    return out_l, out_c


def swiglu(h, w1, w3, w2):
    return (jax.nn.silu(h @ w1) * (h @ w3)) @ w2


def moe_swiglu(h, router_w, router_b, w1, w3, w2):
    logits = (h @ router_w).astype(F32) + router_b.astype(F32)
    top_vals, top_idx = lax.top_k(logits, TOP_K)
    weights = jax.nn.softmax(top_vals, axis=-1)
    gate = jnp.sum(jax.nn.one_hot(top_idx, N_EXPERTS, dtype=F32) * weights[..., None], axis=-2)
    gate = gate.astype(h.dtype)
    out = jnp.zeros_like(h)
    for e in range(N_EXPERTS):
        out = out + gate[..., e:e + 1] * swiglu(h, w1[e], w3[e], w2[e])
    return out


def channel_mixer(h, i, ffn_w1, ffn_w3, ffn_w2, router_w, router_b, moe_w1, moe_w3, moe_w2):
    j = i // 2
    if i % 2 == 0:
        return swiglu(h, ffn_w1[j], ffn_w3[j], ffn_w2[j])
    return moe_swiglu(h, router_w[j], router_b[j], moe_w1[j], moe_w3[j], moe_w2[j])


def setup_inputs(seed: int = 0) -> dict:
    key = jax.random.key(seed)
    ks = iter(jax.random.split(key, 48))

    def nrm(shape, scale):
        return jax.random.normal(next(ks), shape, F32) * scale

    def uni(shape, lo, hi):
        return jax.random.uniform(next(ks), shape, F32, lo, hi)

    n_dense = (DEPTH + 1) // 2
    n_moe = DEPTH // 2
    gam = 1.0 - 2.0 ** (-5.0 - np.arange(RET_HEADS))
    ret_logit = jnp.asarray(np.log(gam / (1.0 - gam)), F32)
    dt = jnp.exp(uni((DEPTH, 2, SSD_HEADS), float(np.log(1e-3)), float(np.log(1e-1))))
    a0 = uni((DEPTH, 2, LRU_WIDTH), 0.9, 0.999) ** (1.0 / LRU_C)
    inp = {}
    inp['x'] = nrm((BATCH, SEQ, D_MODEL), 1.0)
    inp['c'] = nrm((BATCH, D_MODEL), 1.0)
    inp['ctx'] = nrm((BATCH, CTX_LEN, D_MODEL), 1.0)
    inp['c_ctx'] = nrm((D_MODEL,), 1.0)
    inp['w_mod'] = nrm((DEPTH, D_MODEL, 6 * D_MODEL), 0.5 * D_MODEL ** -0.5)
    inp['b_mod'] = nrm((DEPTH, 6 * D_MODEL), 0.02)
    inp['norm1_g'] = 1.0 + nrm((DEPTH, D_MODEL), 0.02)
    inp['norm2_g'] = 1.0 + nrm((DEPTH, D_MODEL), 0.02)
    inp['w_in'] = nrm((DEPTH, D_MODEL, D_IN), D_MODEL ** -0.5)
    inp['ret_decay'] = ret_logit + nrm((DEPTH, 2, RET_HEADS), 0.1)
    inp['ret_gn_g'] = 1.0 + nrm((DEPTH, RET_V), 0.02)
    inp['ssd_conv_w'] = nrm((DEPTH, CONV_W, SSD_CONV_DIM), CONV_W ** -0.5)
    inp['ssd_conv_b'] = nrm((DEPTH, SSD_CONV_DIM), 0.02)
    inp['ssd_dt_bias'] = dt + jnp.log(-jnp.expm1(-dt))
    inp['ssd_a_log'] = jnp.log(uni((DEPTH, 2, SSD_HEADS), 1.0, 16.0))
    inp['ssd_d'] = 1.0 + nrm((DEPTH, SSD_HEADS), 0.02)
    inp['ssd_norm_g'] = 1.0 + nrm((DEPTH, SSD_INNER), 0.02)
    inp['lru_conv_w'] = nrm((DEPTH, CONV_W, LRU_WIDTH), CONV_W ** -0.5)
    inp['lru_conv_b'] = nrm((DEPTH, LRU_WIDTH), 0.02)
    inp['lru_gate_w'] = nrm((DEPTH, 2, 2, LRU_BLOCKS, LRU_BW, LRU_BW), LRU_BW ** -0.5)
    inp['lru_gate_b'] = nrm((DEPTH, 2, 2, LRU_WIDTH), 0.02)
    inp['lru_lambda'] = jnp.log(a0) - jnp.log1p(-a0)
    inp['w_branch'] = nrm((DEPTH, N_BRANCH, BRANCH_W, D_MODEL), BRANCH_W ** -0.5)
    inp['w_out'] = nrm((DEPTH, D_MODEL, D_MODEL), D_MODEL ** -0.5)
    inp['ffn_w1'] = nrm((n_dense, D_MODEL, D_FF), D_MODEL ** -0.5)
    inp['ffn_w3'] = nrm((n_dense, D_MODEL, D_FF), D_MODEL ** -0.5)
    inp['ffn_w2'] = nrm((n_dense, D_FF, D_MODEL), D_FF ** -0.5)
    inp['router_w'] = nrm((n_moe, D_MODEL, N_EXPERTS), D_MODEL ** -0.5)
    inp['router_b'] = nrm((n_moe, N_EXPERTS), 0.01)
    inp['moe_w1'] = nrm((n_moe, N_EXPERTS, D_MODEL, D_FF_EXPERT), D_MODEL ** -0.5)
    inp['moe_w3'] = nrm((n_moe, N_EXPERTS, D_MODEL, D_FF_EXPERT), D_MODEL ** -0.5)
    inp['moe_w2'] = nrm((n_moe, N_EXPERTS, D_FF_EXPERT, D_MODEL), D_FF_EXPERT ** -0.5)
    inp['final_g'] = 1.0 + nrm((D_MODEL,), 0.02)
    return inp


def reference(x, c, ctx, c_ctx, w_mod, b_mod, norm1_g, norm2_g, w_in, ret_decay, ret_gn_g,
              ssd_conv_w, ssd_conv_b, ssd_dt_bias, ssd_a_log, ssd_d, ssd_norm_g,
              lru_conv_w, lru_conv_b, lru_gate_w, lru_gate_b, lru_lambda, w_branch, w_out,
              ffn_w1, ffn_w3, ffn_w2, router_w, router_b, moe_w1, moe_w3, moe_w2, final_g):
    cos_l, sin_l = axial_rotary(x.shape[1])
    cond_l = jax.nn.silu(c)[:, None, :]
    cond_c = jax.nn.silu(c_ctx)[None, None, :]
    xl, xc = x, ctx
    for i in range(DEPTH):
        last = i == DEPTH - 1
        sh1_l, sc1_l, g1_l, sh2_l, sc2_l, g2_l = jnp.split(cond_l @ w_mod[i] + b_mod[i], 6, axis=-1)
        sh1_c, sc1_c, g1_c, sh2_c, sc2_c, g2_c = jnp.split(cond_c @ w_mod[i] + b_mod[i], 6, axis=-1)
        hl = rmsnorm(xl, norm1_g[i]) * (1.0 + sc1_l) + sh1_l
        hc = rmsnorm(xc, norm1_g[i]) * (1.0 + sc1_c) + sh1_c
        yl, yc = mixer_forward(hl, hc, cos_l, sin_l, w_in[i], ret_decay[i], ret_gn_g[i],
                               ssd_conv_w[i], ssd_conv_b[i], ssd_dt_bias[i], ssd_a_log[i], ssd_d[i],
                               ssd_norm_g[i], lru_conv_w[i], lru_conv_b[i], lru_gate_w[i],
                               lru_gate_b[i], lru_lambda[i], w_branch[i], w_out[i], not last)
        xl = xl + g1_l * yl
        hl = rmsnorm(xl, norm2_g[i]) * (1.0 + sc2_l) + sh2_l
        xl = xl + g2_l * channel_mixer(hl, i, ffn_w1, ffn_w3, ffn_w2, router_w, router_b,
                                       moe_w1, moe_w3, moe_w2)
        if not last:
            xc = xc + g1_c * yc
            hc = rmsnorm(xc, norm2_g[i]) * (1.0 + sc2_c) + sh2_c
            xc = xc + g2_c * channel_mixer(hc, i, ffn_w1, ffn_w3, ffn_w2, router_w, router_b,
                                           moe_w1, moe_w3, moe_w2)
    return rmsnorm(xl, final_g)
```

```python
import contextlib
import os
import numpy as np
import concourse.bass as bass
import concourse.mybir as mybir
from concourse.bass_utils import run_bass_kernel_spmd

F32 = mybir.dt.float32
BF16 = mybir.dt.bfloat16
AF = mybir.ActivationFunctionType
ALU = mybir.AluOpType
AX = mybir.AxisListType

D = 2048
KT = 16
EPS = 1e-6
NEG = -30000.0


class Sched:
    ENG = ("pe", "act", "dve", "pool", "sp")
    NDS = 8

    def __init__(self, nc, stack):
        self.nc = nc
        self.ops = []
        self.cnt = {e: 0 for e in self.ENG}
        self.dcnt = {e: 0 for e in self.ENG}
        self.seen = {e: {} for e in self.ENG}
        self.last_w = {}
        self.readers = {}
        self.bar = {}
        self.sems = {}
        for e in ("pe", "act", "dve", "pool"):
            self.sems[("c", e)] = stack.enter_context(nc.semaphore("c_" + e))
        for e in ("sp", "pool", "act"):
            for s in range(self.NDS):
                self.sems[("d", e, s)] = stack.enter_context(nc.semaphore("d_%s_%d" % (e, s)))
        self.nops = 0

    def op(self, eng, fn, reads=(), writes=(), dma=False):
        need = dict(self.bar)
        if dma:
            j = self.dcnt[eng]
            self.dcnt[eng] += 1
            sk = ("d", eng, j % self.NDS)
            tok = (sk, 16 * (j // self.NDS + 1))
            if j >= self.NDS:
                need[sk] = max(need.get(sk, 0), 16 * (j // self.NDS))
        else:
            self.cnt[eng] += 1
            tok = (("c", eng), self.cnt[eng])

        def add(t):
            if t is None:
                return
            (sk, v), peng, pdma = t
            if peng == "pe" and eng == "pe" and not pdma and not dma:
                return
            if need.get(sk, 0) < v:
                need[sk] = v

        for k in reads:
            add(self.last_w.get(k))
        for k in writes:
            add(self.last_w.get(k))
            for t in self.readers.get(k, ()):
                add(t)
        wl = []
        seen = self.seen[eng]
        for sk, v in need.items():
            if seen.get(sk, 0) < v:
                seen[sk] = v
                wl.append((sk, v))
        me = (tok, eng, dma)
        for k in reads:
            self.readers.setdefault(k, []).append(me)
        for k in writes:
            self.last_w[k] = me
            self.readers[k] = []
        self.ops.append((eng, fn, wl, tok, dma))
        self.nops += 1

    def barrier(self):
        bar = {}
        for e in ("pe", "act", "dve", "pool"):
            if self.cnt[e]:
                bar[("c", e)] = self.cnt[e]
        for e in ("sp", "pool", "act"):
            jt = self.dcnt[e]
            for s in range(self.NDS):
                k = (jt - s + self.NDS - 1) // self.NDS if jt > s else 0
                if k:
                    bar[("d", e, s)] = 16 * k
        self.bar = bar

    def maybe_flush(self, limit=16000, dlimit=48):
        if len(self.ops) >= limit or sum(1 for o in self.ops if o[4]) >= dlimit:
            self.flush()

    def flush(self, final=False):
        ops = self.ops
        self.ops = []
        per = {e: [o for o in ops if o[0] == e] for e in self.ENG}
        sems = self.sems
        fin = {}
        if final:
            self.barrier()
            fin = self.bar

        def mk(ename):
            lst = per[ename]

            def body(e):
                for (_, fn, wl, tok, dma) in lst:
                    for sk, v in wl:
                        e.wait_ge(sems[sk], v)
                    ins = fn(e)
                    ins.then_inc(sems[tok[0]], 16 if dma else 1)
                if final and ename in ("sp", "pool", "act"):
                    for sk, v in fin.items():
                        if sk[0] == "d" and sk[1] == ename and self.seen[ename].get(sk, 0) < v:
                            e.wait_ge(sems[sk], v)
            return body

        with self.nc.Block() as block:
            if per["pe"]:
                block.tensor(mk("pe"))
            if per["act"] or final:
                block.scalar(mk("act"))
            if per["dve"]:
                block.vector(mk("dve"))
            if per["pool"] or final:
                block.gpsimd(mk("pool"))
            if per["sp"] or final:
                block.sync(mk("sp"))


def make_consts(Tl):
    p = np.arange(128)
    c = {}
    c["IDF"] = np.eye(128)
    c["UF"] = (p[:, None] <= p[None, :]) * 1.0
    c["UB"] = (p[:, None] >= p[None, :]) * 1.0
    c["NEGF"] = np.where(p[None, :] < p[:, None], NEG, 0.0)
    c["NEGB"] = np.where(p[None, :] > p[:, None], NEG, 0.0)
    c["ONES"] = np.ones((128, 128))
    c["DIFFF"] = np.maximum(p[None, :] - p[:, None], 0.0)
    c["DIFFB"] = np.maximum(p[:, None] - p[None, :], 0.0)
    sc = 128.0 ** -0.5
    c["TRIF"] = (p[None, :] >= p[:, None]) * sc
    c["TRIB"] = (p[:, None] >= p[None, :]) * sc
    rt = np.zeros((128, 128))
    for d in range(64):
        rt[d + 64, d] = -1.0
        rt[d, d + 64] = 1.0
    c["RT"] = rt
    c["IDXQF"] = np.tile((p + 1.0)[None, :], (128, 1))
    c["IDXQB"] = np.tile((128.0 - p)[None, :], (128, 1))
    idxk = np.zeros((128, 128))
    idxk[:, 0:8] = (127.0 - p)[:, None]
    idxk[:, 8:16] = p[:, None] * 1.0
    c["IDXK"] = idxk
    names = list(c.keys())
    arr = np.concatenate([c[n] for n in names], axis=1).astype(np.float32)
    offs = {n: i * 128 for i, n in enumerate(names)}
    t = np.arange(Tl)
    row = (t // 64).astype(np.float32)
    col = (t % 64).astype(np.float32)
    inv = (10000.0 ** (-np.arange(32, dtype=np.float32) / 32)).astype(np.float32)
    ang = np.concatenate([row[:, None] * inv, col[:, None] * inv], axis=-1)
    ang = ang.astype(np.float32)
    cos2 = np.concatenate([np.cos(ang), np.cos(ang)], axis=-1).T
    sin2 = np.concatenate([np.sin(ang), np.sin(ang)], axis=-1).T
    rot = np.ascontiguousarray(np.stack([cos2, sin2], axis=1)).astype(np.float32)
    return arr, offs, rot


W_OFF = dict(q=0, k=1024, v=2048, g=4096, z=6144, xbc=8192, dt=11264, lg=11328, rec=13376, mg=15424)
D_IN = 21568


def build(Tc, Tl, depth, n_dense, n_moe, dff=5632, nexp=8, dffe=4096, debug=False, nlayers=None, stop_phase=None):
    T = Tc + Tl
    TP = T + 6
    NCH_C = Tc // 128
    NCH_L = Tl // 128
    carr, coff, _ = make_consts(Tl)
    NCONST = carr.shape[1]
    HID_MOE = nexp * dffe
    HIDMAX = max(dff, HID_MOE if n_moe else 0)

    nc = bass.Bass("TRN2", target_bir_lowering=False)
    _uid = [0]

    def U(name):
        _uid[0] += 1
        return "%s_%d" % (name, _uid[0])

    def din(name, shape):
        return nc.dram_tensor(name, list(shape), F32, kind="ExternalInput").ap()

    def dscr(name, shape, dt):
        return nc.dram_tensor(name, list(shape), dt, kind="ExternalOutput").ap()

    x_in = din("x", [Tl, D]); ctx_in = din("ctx", [Tc, D]); c_in = din("c", [1, D]); cc_in = din("c_ctx", [1, D])
    w_mod = [din("w_mod%d" % l, [D, 6 * D]) for l in range(depth)]; b_mod = din("b_mod", [depth, 6 * D])
    norm1_g = din("norm1_g", [depth, D]); norm2_g = din("norm2_g", [depth, D])
    w_in = [din("w_in%d" % l, [D, D_IN]) for l in range(depth)]
    ret_decay = din("ret_decay", [depth, 16]); ret_gn_g = din("ret_gn_g", [depth, D])
    ssd_conv_w = din("ssd_conv_w", [depth, 4, 3072]); ssd_conv_b = din("ssd_conv_b", [depth, 3072])
    ssd_dt_bias = din("ssd_dt_bias", [depth, 64]); ssd_a_log = din("ssd_a_log", [depth, 64])
    ssd_d = din("ssd_d", [depth, 32]); ssd_norm_g = din("ssd_norm_g", [depth, D])
    lru_conv_w = din("lru_conv_w", [depth, 4, D]); lru_conv_b = din("lru_conv_b", [depth, D])
    lru_gate_w = din("lru_gate_w", [depth, 64, 128, 128]); lru_gate_b = din("lru_gate_b", [depth, 4, D])
    lru_lambda = din("lru_lambda", [depth, 2, D])
    w_branch = [[din("w_branch%d_%d" % (l, k), [D, D]) for k in range(3)] for l in range(depth)]
    w_out = [din("w_out%d" % l, [D, D]) for l in range(depth)]
    ffn_w1 = [din("ffn_w1_%d" % l, [D, dff]) for l in range(n_dense)]; ffn_w3 = [din("ffn_w3_%d" % l, [D, dff]) for l in range(n_dense)]
    ffn_w2 = [din("ffn_w2_%d" % l, [dff, D]) for l in range(n_dense)]
    if n_moe:
        router_w = din("router_w", [n_moe, D, nexp]); router_b = din("router_b", [n_moe, nexp])
        moe_w1 = [[din("moe_w1_%d_%d" % (l, e), [D, dffe]) for e in range(nexp)] for l in range(n_moe)]
        moe_w3 = [[din("moe_w3_%d_%d" % (l, e), [D, dffe]) for e in range(nexp)] for l in range(n_moe)]
        moe_w2 = [[din("moe_w2_%d_%d" % (l, e), [dffe, D]) for e in range(nexp)] for l in range(n_moe)]
    final_g = din("final_g", [1, D])
    consts_in = din("consts", [128, NCONST]); rot_in = din("rot", [128, 2, Tl])
    out = nc.dram_tensor("out", [Tl, D], F32, kind="ExternalOutput").ap()

    X = dscr("X", [T, D], F32)
    MODD = dscr("MODD", [depth, 2, 6 * D], F32)
    PQK = dscr("PQK", [2048, T], BF16)
    PX = dscr("PX", [128, 24, TP], F32)
    PREC = dscr("PREC", [128, 16, TP], F32)
    PGG = dscr("PGG", [2048, T], BF16)
    PSG = dscr("PSG", [6144, T], BF16)
    PV = dscr("PV", [T, D], BF16); PG = dscr("PG", [T, D], BF16); PZ = dscr("PZ", [T, D], BF16)
    PDT = dscr("PDT", [T, 64], F32)
    YRET = dscr("YRET", [T, D], F32); YSSD = dscr("YSSD", [T, D], F32)
    HF = dscr("HF", [2048, T], F32)
    UT = dscr("UT", [3, 2048, T], BF16)
    AHR = 4096
    AHL = [dscr("AH%d" % i, [min(AHR, HIDMAX - i * AHR), T], BF16) for i in range((HIDMAX + AHR - 1) // AHR)]

    def AHv(r0, nr, t0, n):
        i = r0 // AHR
        assert (r0 + nr - 1) // AHR == i
        return AHL[i][r0 - i * AHR:r0 - i * AHR + nr, t0:t0 + n]
    GATET = dscr("GATET", [8, T], F32)

    def colof(tok):
        return tok + 2 if tok < Tc else tok + 4

    seqs = [(0, Tc), (Tc, Tl)]

    def tok_blocks(skip_ctx=False, bs=512):
        res = []
        for (s0, sn) in seqs:
            if skip_ctx and s0 == 0:
                continue
            o = 0
            while o < sn:
                n = min(bs, sn - o)
                res.append((s0 + o, n))
                o += n
        return res

    with contextlib.ExitStack() as gst:
        S = Sched(nc, gst)

        def gsb(name, shape, dt):
            return gst.enter_context(nc.sbuf_tensor(U(name), list(shape), dt))

        def MM(o, lhsT, rhs, start, stop, r, w):
            S.op("pe", lambda e: e.matmul(o, lhsT=lhsT, rhs=rhs, start=start, stop=stop), r, w)

        def TR(o, in_, ident, r, w):
            S.op("pe", lambda e: e.transpose(o, in_, ident), r, w)

        def ACT(o, in_, func, r, w, bias=None, scale=None, accum_out=None):
            kw = {}
            if bias is not None:
                kw["bias"] = bias
            if scale is not None:
                kw["scale"] = scale
            if accum_out is not None:
                kw["accum_out"] = accum_out
            S.op("act", lambda e: e.activation(out=o, in_=in_, func=func, **kw), r, w)

        def TT(eng, o, in0, in1, op, r, w):
            S.op(eng, lambda e: e.tensor_tensor(out=o, in0=in0, in1=in1, op=op), r, w)

        def TS(eng, o, in0, s1, s2, op0, op1, r, w, accum_out=None):
            if accum_out is not None:
                S.op(eng, lambda e: e.tensor_scalar(out=o, in0=in0, scalar1=s1, scalar2=s2, op0=op0, op1=op1, accum_out=accum_out), r, w)
            elif s2 is None:
                S.op(eng, lambda e: e.tensor_scalar(out=o, in0=in0, scalar1=s1, scalar2=None, op0=op0), r, w)
            else:
                S.op(eng, lambda e: e.tensor_scalar(out=o, in0=in0, scalar1=s1, scalar2=s2, op0=op0, op1=op1), r, w)

        def STT(o, in0, scalar, in1, op0, op1, r, w):
            S.op("dve", lambda e: e.scalar_tensor_tensor(out=o, in0=in0, scalar=scalar, in1=in1, op0=op0, op1=op1), r, w)

        def CP(eng, o, in_, r, w):
            if eng == "act":
                S.op("act", lambda e: e.copy(out=o, in_=in_), r, w)
            else:
                S.op(eng, lambda e: e.tensor_copy(out=o, in_=in_), r, w)

        def MEMSET(eng, o, val, w):
            S.op(eng, lambda e: e.memset(o, val), (), w)

        def RED(o, in_, op, r, w):
            S.op("dve", lambda e: e.tensor_reduce(out=o, in_=in_, axis=AX.X, op=op), r, w)

        def DMA(eng, o, in_, r, w, **kw):
            S.op(eng, lambda e: e.dma_start(out=o, in_=in_, **kw), r, w, dma=True)

        CONST = gsb("CONST", [128, NCONST], F32)
        CB = gsb("CONSTB", [128, 2 * 128], BF16)
        psb = [gst.enter_context(nc.psum_tensor("ps%d" % i, [128, 512], F32)) for i in range(8)]
        ps_i = [0]

        def PS():
            i = ps_i[0] % 7
            ps_i[0] += 1
            return psb[i], ("ps", i)

        def PSX():
            return psb[7], ("ps", 7)

        def CO(name):
            o = coff[name]
            return CONST[:, o:o + 128]

        DMA("sp", CONST[:], consts_in[:, :], [], ["CONST"])
        CP("dve", CB[:, 0:128], CO("IDF"), ["CONST"], ["CB"])
        CP("dve", CB[:, 128:256], CO("RT"), ["CONST"], ["CB"])
        IDB = CB[:, 0:128]
        RTB = CB[:, 128:256]

        for r0 in range(0, Tc, 256):
            DMA("sp", X[r0:r0 + 256, :] if r0 + 256 <= Tc else X[r0:Tc, :], ctx_in[r0:min(r0 + 256, Tc), :], [], ["X"])
        for r0 in range(0, Tl, 256):
            DMA("sp", X[Tc + r0:Tc + r0 + 256, :], x_in[r0:r0 + 256, :], [], ["X"])

        with contextlib.ExitStack() as st:
            def sb(name, shape, dt):
                return st.enter_context(nc.sbuf_tensor(U(name), list(shape), dt))
            crow = sb("crow", [2, D], F32)
            cT = sb("cT", [128, KT, 2], BF16)
            wm = [sb("wm%d" % i, [128, KT, 512], BF16) for i in range(2)]
            bm = sb("bm", [2, 6 * D], F32)
            mo = sb("mo", [2, 6 * D], F32)
            DMA("sp", crow[0:1, :], c_in[0:1, :], [], ["crow"])
            DMA("sp", crow[1:2, :], cc_in[0:1, :], [], ["crow"])
            ACT(crow[:], crow[:], AF.Silu, ["crow"], ["crow"])
            p, pk = PS()
            for kt in range(KT):
                TR(p[:, kt * 2:(kt + 1) * 2], crow[0:2, kt * 128:(kt + 1) * 128], CO("IDF")[0:2, 0:2], ["crow", "CONST"], [pk])
            CP("dve", cT[:].rearrange("p k o -> p (k o)"), p[:, 0:2 * KT], [pk], ["cT"])
            for li in range(depth):
                DMA("sp", bm[:], b_mod[li:li + 1, :].partition_broadcast(2), [], ["bm"])
                for cb in range(6 * D // 512):
                    wt = wm[cb % 2]; wk = ("wm", cb % 2)
                    DMA("pool", wt[:], w_mod[li][:, cb * 512:(cb + 1) * 512].rearrange("(kt p) c -> p kt c", p=128), [], [wk])
                    p, pk = PS()
                    for kt in range(KT):
                        MM(p[0:2, :], cT[:, kt, :], wt[:, kt, :], kt == 0, kt == KT - 1, [wk, "cT"], [pk])
                    TT("dve", mo[:, cb * 512:(cb + 1) * 512], p[0:2, :], bm[:, cb * 512:(cb + 1) * 512], ALU.add, [pk, "bm"], ["mo"])
                DMA("sp", MODD[li, :, :], mo[:], ["mo"], [("MODD", li)])
            S.flush()
        S.barrier()

        def phase_A(HT, li, which, gain_ap, skip_ctx, router=None):
            with contextlib.ExitStack() as st:
                def sb(name, shape, dt):
                    return st.enter_context(nc.sbuf_tensor(U(name), list(shape), dt))
                GM = sb("GM", [128, D], F32); SH = sb("SH", [128, D], F32)
                xt = [sb("xt%d" % i, [128, D], F32) for i in range(2)]
                hb = [sb("hb%d" % i, [128, D], BF16) for i in range(2)]
                tmp = sb("tmpA", [128, D], F32)
                st8 = sb("st8", [128, 8], F32)
                if router is not None:
                    rw_ap, rb_ap = router
                    rw = sb("rw", [128, KT, 8], F32); rb = sb("rb", [128, 8], F32)
                    hT32 = sb("hT32", [128, KT, 128], F32)
                    lg = sb("lgt", [128, 8], F32); l2 = sb("lgt2", [128, 8], F32)
                    m1 = sb("m1", [128, 8], F32); m2 = sb("m2", [128, 8], F32)
                    gt = sb("gt", [128, 8], F32); gT = sb("gT", [8, 128], F32)
                    DMA("sp", rw[:], rw_ap.rearrange("(kt p) e -> p kt e", p=128), [], ["rw"])
                    DMA("sp", rb[:], rb_ap.partition_broadcast(128), [], ["rb"])
                for c0 in (0, Tc + 2, T + 4):
                    MEMSET("pool", HT[:, :, c0:c0 + 2], 0.0, [("HT", "pad", c0)])
                for (s0, sn) in seqs:
                    if skip_ctx and s0 == 0:
                        continue
                    row = 1 if s0 == 0 else 0
                    sh_o = (0 if which == 0 else 3) * D
                    sc_o = sh_o + D
                    DMA("sp", SH[:], MODD[li, row:row + 1, sh_o:sh_o + D].partition_broadcast(128), [("MODD", li)], ["SH"])
                    DMA("sp", GM[:], MODD[li, row:row + 1, sc_o:sc_o + D].partition_broadcast(128), [("MODD", li)], ["GM"])
                    DMA("sp", tmp[:], gain_ap.partition_broadcast(128), [], ["tmpA"])
                    STT(GM[:], GM[:], 1.0, tmp[:], ALU.add, ALU.mult, ["GM", "tmpA"], ["GM"])
                    for ti in range(sn // 128):
                        t0 = s0 + ti * 128
                        b = ti % 2
                        xk = ("xt", b); hk = ("hb", b)
                        DMA("sp", xt[b][:], X[t0:t0 + 128, :], ["X"], [xk])
                        ACT(tmp[:], xt[b][:], AF.Square, [xk], ["tmpA", "st8"], accum_out=st8[:, 0:1])
                        ACT(st8[:, 1:2], st8[:, 0:1], AF.Ln, ["st8"], ["st8"], bias=EPS, scale=1.0 / D)
                        ACT(st8[:, 2:3], st8[:, 1:2], AF.Exp, ["st8"], ["st8"], scale=-0.5)
                        STT(tmp[:], xt[b][:], st8[:, 2:3], GM[:], ALU.mult, ALU.mult, [xk, "st8", "GM"], ["tmpA"])
                        if router is None:
                            TT("pool", hb[b][:], tmp[:], SH[:], ALU.add, ["tmpA", "SH"], [hk])
                        else:
                            TT("dve", tmp[:], tmp[:], SH[:], ALU.add, ["tmpA", "SH"], ["tmpA"])
                            CP("pool", hb[b][:], tmp[:], ["tmpA"], [hk])
                        c0 = colof(t0)
                        for half in range(2):
                            p, pk = PS()
                            pb = p[:].bitcast(BF16)
                            for j in range(8):
                                kt = half * 8 + j
                                TR(pb[:, j * 128:(j + 1) * 128], hb[b][:, kt * 128:(kt + 1) * 128], IDB, [hk, "CB"], [pk])
                            CP("act" if half == 0 else "dve", HT[:, half * 8:(half + 1) * 8, c0:c0 + 128],
                               pb[:, 0:1024].rearrange("p (k t) -> p k t", t=128), [pk], [("HT", t0)])
                        if router is not None:
                            for q4 in range(4):
                                p, pk = PS()
                                for j in range(4):
                                    kt = q4 * 4 + j
                                    TR(p[:, j * 128:(j + 1) * 128], tmp[:, kt * 128:(kt + 1) * 128], CO("IDF"), ["tmpA", "CONST"], [pk])
                                CP("act", hT32[:, q4 * 4:(q4 + 1) * 4, :], p[:].rearrange("p (k t) -> p k t", t=128), [pk], ["hT32"])
                            p, pk = PS()
                            for kt in range(KT):
                                MM(p[:, 0:8], hT32[:, kt, :], rw[:, kt, :], kt == 0, kt == KT - 1, ["hT32", "rw"], [pk])
                            TT("dve", lg[:], p[:, 0:8], rb[:], ALU.add, [pk, "rb"], ["lg"])
                            RED(st8[:, 3:4], lg[:], ALU.max, ["lg"], ["st8r"])
                            TS("dve", m1[:], lg[:], st8[:, 3:4], None, ALU.is_equal, None, ["lg", "st8r"], ["m1"])
                            STT(l2[:], m1[:], NEG, lg[:], ALU.mult, ALU.add, ["m1", "lg"], ["l2"])
                            RED(st8[:, 4:5], l2[:], ALU.max, ["l2"], ["st8r"])
                            TS("dve", m2[:], l2[:], st8[:, 4:5], None, ALU.is_equal, None, ["l2", "st8r"], ["m2"])
                            TT("dve", st8[:, 5:6], st8[:, 4:5], st8[:, 3:4], ALU.subtract, ["st8r"], ["st8r"])
                            ACT(st8[:, 5:6], st8[:, 5:6], AF.Exp, ["st8r"], ["st8r"])
                            TS("dve", st8[:, 5:6], st8[:, 5:6], 1.0, None, ALU.add, None, ["st8r"], ["st8r"])
                            S.op("dve", lambda e: e.reciprocal(out=st8[:, 6:7], in_=st8[:, 5:6]), ["st8r"], ["st8r"])
                            TS("dve", st8[:, 7:8], st8[:, 6:7], -1.0, 1.0, ALU.mult, ALU.add, ["st8r"], ["st8r"])
                            TS("dve", gt[:], m1[:], st8[:, 6:7], None, ALU.mult, None, ["m1", "st8r"], ["gt"])
                            STT(gt[:], m2[:], st8[:, 7:8], gt[:], ALU.mult, ALU.add, ["m2", "st8r", "gt"], ["gt"])
                            p, pk = PS()
                            TR(p[0:8, 0:128], gt[:], CO("IDF"), ["gt", "CONST"], [pk])
                            CP("act", gT[:], p[0:8, 0:128], [pk], ["gT"])
                            DMA("sp", GATET[:, t0:t0 + 128], gT[:], ["gT"], ["GATET"])
                S.flush()
            S.barrier()

        def phase_P(HT, li):
            with contextlib.ExitStack() as st:
                def sb(name, shape, dt):
                    return st.enter_context(nc.sbuf_tensor(U(name), list(shape), dt))
                wf = [sb("wf%d" % i, [128, KT, 128], BF16) for i in range(3)]
                wtm = [sb("wtm%d" % i, [128, KT, 512], BF16) for i in range(2)]
                ev32 = [sb("ev32_%d" % i, [128, 512], F32) for i in range(3)]
                ev16 = [sb("ev16_%d" % i, [128, 512], BF16) for i in range(3)]
                cnt = [0]
                colblocks = []
                o = 0
                while o < TP:
                    n = min(512, TP - o)
                    colblocks.append((o, n))
                    o += n
                tblocks = tok_blocks()
                wcnt = [0]

                def fm_tile(wcol, dst, drow, mode):
                    S.maybe_flush()
                    wi = wcnt[0] % 3; wcnt[0] += 1
                    wt = wf[wi]; wk = ("wf", wi)
                    DMA("pool", wt[:], w_in[li][:, wcol:wcol + 128].rearrange("(kt p) c -> p kt c", p=128), [], [wk])
                    if mode == "pad32":
                        blks = [(c0, n, c0) for (c0, n) in colblocks]
                    else:
                        blks = [(colof(t0), n, t0) for (t0, n) in tblocks]
                    for (c0, n, d0) in blks:
                        p, pk = PS()
                        for kt in range(KT):
                            MM(p[:, 0:n], wt[:, kt, :], HT[:, kt, c0:c0 + n], kt == 0, kt == KT - 1, [wk, "HTall"], [pk])
                        i = cnt[0] % 3; cnt[0] += 1
                        if mode == "pad32":
                            ek = ("ev32", i)
                            if i % 2 == 0:
                                CP("act", ev32[i][:, 0:n], p[:, 0:n], [pk], [ek])
                            else:
                                CP("dve", ev32[i][:, 0:n], p[:, 0:n], [pk], [ek])
                            DMA("sp", dst[:, drow // 128, d0:d0 + n], ev32[i][:, 0:n], [ek], [dst.name])
                        else:
                            ek = ("ev16", i)
                            if mode == "bf":
                                if i % 2 == 0:
                                    CP("act", ev16[i][:, 0:n], p[:, 0:n], [pk], [ek])
                                else:
                                    CP("dve", ev16[i][:, 0:n], p[:, 0:n], [pk], [ek])
                            else:
                                ACT(ev16[i][:, 0:n], p[:, 0:n], AF.Sigmoid if mode == "sig" else AF.Gelu, [pk], [ek])
                            DMA("sp", dst[drow:drow + 128, d0:d0 + n], ev16[i][:, 0:n], [ek], [dst.name])

                def tm_block(wcol, ncols, dst, dcol, mode):
                    S.maybe_flush()
                    wi = wcnt[0] % 2; wcnt[0] += 1
                    wt = wtm[wi]; wk = ("wtm", wi)
                    DMA("pool", wt[:, :, 0:ncols], w_in[li][:, wcol:wcol + ncols].rearrange("(kt p) c -> p kt c", p=128), [], [wk])
                    for t0 in range(0, T, 128):
                        c0 = colof(t0)
                        p, pk = PS()
                        for kt in range(KT):
                            MM(p[:, 0:ncols], HT[:, kt, c0:c0 + 128], wt[:, kt, 0:ncols], kt == 0, kt == KT - 1, [wk, "HTall"], [pk])
                        i = cnt[0] % 3; cnt[0] += 1
                        if mode == "f32":
                            ek = ("ev32", i)
                            CP("dve", ev32[i][:, 0:ncols], p[:, 0:ncols], [pk], [ek])
                            DMA("sp", dst[t0:t0 + 128, dcol:dcol + ncols], ev32[i][:, 0:ncols], [ek], [dst.name])
                        else:
                            ek = ("ev16", i)
                            if mode == "silu":
                                ACT(ev16[i][:, 0:ncols], p[:, 0:ncols], AF.Silu, [pk], [ek])
                            elif i % 2 == 0:
                                CP("act", ev16[i][:, 0:ncols], p[:, 0:ncols], [pk], [ek])
                            else:
                                CP("dve", ev16[i][:, 0:ncols], p[:, 0:ncols], [pk], [ek])
                            DMA("sp", dst[t0:t0 + 128, dcol:dcol + ncols], ev16[i][:, 0:ncols], [ek], [dst.name])

                for j in range(8):
                    fm_tile(W_OFF["q"] + j * 128, PQK, j * 128, "bf")
                for j in range(8):
                    fm_tile(W_OFF["k"] + j * 128, PQK, 1024 + j * 128, "bf")
                for j in range(24):
                    fm_tile(W_OFF["xbc"] + j * 128, PX, j * 128, "pad32")
                for j in range(16):
                    fm_tile(W_OFF["rec"] + j * 128, PREC, j * 128, "pad32")
                for j in range(16):
                    fm_tile(W_OFF["lg"] + j * 128, PGG, j * 128, "gelu")
                for j in range(48):
                    fm_tile(W_OFF["mg"] + j * 128, PSG, j * 128, "sig")
                for j in range(4):
                    tm_block(W_OFF["v"] + j * 512, 512, PV, j * 512, "bf")
                for j in range(4):
                    tm_block(W_OFF["g"] + j * 512, 512, PG, j * 512, "silu")
                for j in range(4):
                    tm_block(W_OFF["z"] + j * 512, 512, PZ, j * 512, "silu")
                tm_block(W_OFF["dt"], 64, PDT, 0, "f32")
                S.flush()
            S.barrier()

        def phase_MIX(li):
            with contextlib.ExitStack() as st:
                def sb(name, shape, dt):
                    return st.enter_context(nc.sbuf_tensor(U(name), list(shape), dt))
                lgr = sb("lgr", [128, 16], F32)
                MK = sb("MK", [128, 16, 128], F32)
                QDEC = sb("QDEC", [128, 16, 128], BF16)
                kdec = sb("kdec", [128, 16], F32)
                cdec = sb("cdec", [128, 16], F32)
                tmpc = sb("tmpc", [128, 128], F32)
                Arow = sb("Arow", [128, 64], F32); dtb = sb("dtb", [128, 64], F32); Drow = sb("Drow", [128, 32], F32)
                cw = sb("cw", [128, 24, 5], F32)
                gng = sb("gng", [128, D], F32); sng = sb("sng", [128, D], F32)
                DMA("sp", lgr[:], ret_decay[li:li + 1, :].partition_broadcast(128), [], ["lgr"])
                ACT(lgr[:], lgr[:], AF.Exp, ["lgr"], ["lgr"], scale=-1.0)
                ACT(lgr[:], lgr[:], AF.Ln, ["lgr"], ["lgr"], bias=1.0)
                TS("dve", lgr[:], lgr[:], -1.0, None, ALU.mult, None, ["lgr"], ["lgr"])
                for dr in range(2):
                    for h in range(8):
                        c = dr * 8 + h
                        ACT(tmpc[:], CO("DIFFF" if dr == 0 else "DIFFB"), AF.Exp, ["lgr", "CONST"], ["tmpc"], scale=lgr[:, c:c + 1])
                        TT("dve", MK[:, c, :], tmpc[:], CO("TRIF" if dr == 0 else "TRIB"), ALU.mult, ["tmpc", "CONST"], ["MK"])
                        ACT(QDEC[:, c, :], CO("IDXQF" if dr == 0 else "IDXQB"), AF.Exp, ["lgr", "CONST"], ["QDEC"], scale=lgr[:, c:c + 1])
                TT("dve", kdec[:], lgr[:], CO("IDXK")[:, 0:16], ALU.mult, ["lgr", "CONST"], ["kdec"])
                ACT(kdec[:], kdec[:], AF.Exp, ["kdec"], ["kdec"])
                TS("dve", kdec[:], kdec[:], 128.0 ** -0.5, None, ALU.mult, None, ["kdec"], ["kdec"])
                ACT(cdec[:], lgr[:], AF.Exp, ["lgr"], ["cdec"], scale=128.0)
                DMA("sp", Arow[:], ssd_a_log[li:li + 1, :].partition_broadcast(128), [], ["Arow"])
                ACT(Arow[:], Arow[:], AF.Exp, ["Arow"], ["Arow"])
                TS("dve", Arow[:], Arow[:], -1.0, None, ALU.mult, None, ["Arow"], ["Arow"])
                DMA("sp", dtb[:], ssd_dt_bias[li:li + 1, :].partition_broadcast(128), [], ["dtb"])
                DMA("sp", Drow[:], ssd_d[li:li + 1, :].partition_broadcast(128), [], ["Drow"])
                crows = sb("crows", [5, 3072], F32)
                DMA("sp", crows[0:4, :], ssd_conv_w[li, :, :], [], ["crows"])
                DMA("sp", crows[4:5, :], ssd_conv_b[li:li + 1, :], [], ["crows"])
                p, pk = PS()
                for a in range(24):
                    TR(p[:, a * 5:(a + 1) * 5], crows[0:5, a * 128:(a + 1) * 128], CO("IDF")[0:5, 0:5], ["crows", "CONST"], [pk])
                CP("dve", cw[:].rearrange("p a k -> p (a k)"), p[:, 0:120], [pk], ["cw"])
                DMA("sp", gng[:], ret_gn_g[li:li + 1, :].partition_broadcast(128), [], ["gng"])
                DMA("sp", sng[:], ssd_norm_g[li:li + 1, :].partition_broadcast(128), [], ["sng"])

                Sr = [sb("Sr%d" % d_, [128, 8, 256], F32) for d_ in range(2)]
                Srb = [sb("Srb%d" % d_, [128, 8, 256], BF16) for d_ in range(2)]
                Ss = [sb("Ss%d" % d_, [128, 4, 512], F32) for d_ in range(2)]
                Ssb = [sb("Ssb%d" % d_, [128, 4, 512], BF16) for d_ in range(2)]
                for d_ in range(2):
                    MEMSET("pool", Sr[d_][:], 0.0, [("Sr", d_)])
                    MEMSET("pool", Srb[d_][:], 0.0, [("Srb", d_)])
                    MEMSET("pool", Ss[d_][:], 0.0, [("Ss", d_)])
                    MEMSET("pool", Ssb[d_][:], 0.0, [("Ssb", d_)])

                qk = sb("qk", [128, 16, 128], BF16)
                qkr = sb("qkr", [128, 16, 128], BF16)
                cs = sb("cs", [128, 2, 128], F32)
                tq = sb("tq", [128, 128], F32)
                V = sb("V", [128, D], BF16)
                WT = [sb("WT%d" % i, [128, 128], BF16) for i in range(2)]
                qd = [sb("qd%d" % i, [128, 128], BF16) for i in range(2)]
                kdk = [sb("kdk%d" % i, [128, 128], BF16) for i in range(2)]
                yr = sb("yr", [128, D], F32)
                ys = sb("ys", [128, D], F32)
                ypart = sb("ypart", [128, D], F32)
                xw = sb("xwin", [128, 24, 131], F32)
                acc = sb("cacc", [128, 24, 128], F32)
                xbT = sb("xbT", [128, 24, 128], BF16)
                xs = sb("xs", [128, D], BF16)
                Btok = sb("Btok", [128, 512], BF16)
                dtr = sb("dtr", [128, 32], F32); dtv = sb("dtv", [128, 32], F32); av = sb("av", [128, 32], F32)
                acum = sb("acum", [128, 32], F32); nacum = sb("nacum", [128, 32], F32); ea = sb("ea", [128, 32], F32)
                cdv = sb("cdv", [128, 32], F32); wgt = sb("wgt", [128, 32], F32)
                xwt = sb("xwt", [128, D], BF16)
                abc = [sb("abc%d" % i, [128, 128], F32) for i in range(2)]
                Lm = [sb("Lm%d" % i, [128, 128], F32) for i in range(2)]
                WS = [sb("WS%d" % i, [128, 128], BF16) for i in range(2)]
                cbT = sb("cbT", [128, 4, 128], F32)
                ydg = sb("ydg", [128, 512], F32)
                gz = sb("gz", [128, D], BF16)
                ub = sb("ub", [128, D], BF16)
                uT = sb("uT", [128, KT, 128], BF16)
                st16 = sb("st16", [128, 40], F32)
                sq = sb("sqm", [128, D], F32)

                def chunk(t0, latent, dr, last):
                    lt0 = t0 - Tc
                    c0 = colof(t0)
                    DMA("sp", qk[:], PQK[:, t0:t0 + 128].rearrange("(a p) t -> p a t", p=128), ["PQK"], ["qk"])
                    DMA("sp", V[:], PV[t0:t0 + 128, :], ["PV"], ["V"])
                    if latent:
                        DMA("sp", cs[:], rot_in[:, :, lt0:lt0 + 128], [], ["cs"])
                        for q4 in range(4):
                            p, pk = PS()
                            MM(p[:, :], RTB, qk[:, q4 * 4:(q4 + 1) * 4, :].rearrange("p a t -> p (a t)"), True, True, ["CB", "qk"], [pk])
                            for j in range(4):
                                a = q4 * 4 + j
                                TT("pool", tq[:], qk[:, a, :], cs[:, 0, :], ALU.mult, ["qk", "cs"], ["tq"])
                                TT("dve", p[:, j * 128:(j + 1) * 128], p[:, j * 128:(j + 1) * 128], cs[:, 1, :], ALU.mult, [pk, "cs"], [pk])
                                TT("dve", qkr[:, a, :], p[:, j * 128:(j + 1) * 128], tq[:], ALU.add, [pk, "tq"], [("qkr", a)])
                        Q = qkr
                        qkey = lambda a: ("qkr", a)
                    else:
                        Q = qk
                        qkey = lambda a: "qk"
                    for h in range(8):
                        c = dr * 8 + h
                        b = h % 2
                        psc, psck = PS()
                        MM(psc[:, 0:128], Q[:, 8 + h, :], Q[:, h, :], True, True, [qkey(8 + h), qkey(h)], [psck])
                        TT("dve", WT[b][:], psc[:, 0:128], MK[:, c, :], ALU.mult, [psck, "MK"], [("WT", b)])
                        TT("pool", qd[b][:], Q[:, h, :], QDEC[:, c, :], ALU.mult, [qkey(h), "QDEC"], [("qd", b)])
                        py, pyk = PS()
                        MM(py[:, 0:256], WT[b][:], V[:, h * 256:(h + 1) * 256], True, False, [("WT", b), "V"], [pyk])
                        MM(py[:, 0:256], qd[b][:], Srb[dr][:, h, :], False, True, [("qd", b), ("Srb", dr, h)], [pyk])
                        CP("act", yr[:, h * 256:(h + 1) * 256], py[:, 0:256], [pyk], [("yr", h)])
                        pt, ptk = PS()
                        ptb = pt[:].bitcast(BF16)
                        TR(ptb[:, 0:128], Q[:, 8 + h, :], IDB, [qkey(8 + h), "CB"], [ptk])
                        TS("dve", kdk[b][:], ptb[:, 0:128], kdec[:, c:c + 1], None, ALU.mult, None, [ptk, "kdec"], [("kdk", b)])
                        pkv, pkvk = PS()
                        MM(pkv[:, 0:256], kdk[b][:], V[:, h * 256:(h + 1) * 256], True, True, [("kdk", b), "V"], [pkvk])
                        STT(Sr[dr][:, h, :], Sr[dr][:, h, :], cdec[:, c:c + 1], pkv[:, 0:256], ALU.mult, ALU.add,
                            [("Sr", dr, h), "cdec", pkvk], [("Sr", dr, h)])
                        CP("act", Srb[dr][:, h, :], Sr[dr][:, h, :], [("Sr", dr, h)], [("Srb", dr, h)])
                    DMA("sp", xw[:], PX[:, :, c0 - 2:c0 + 129], ["PX"], ["xw"])
                    DMA("sp", dtr[:], PDT[t0:t0 + 128, dr * 32:(dr + 1) * 32], ["PDT"], ["dtr"])
                    for a in range(24):
                        eng = "dve" if a % 2 == 0 else "pool"
                        TS(eng, acc[:, a, :], xw[:, a, 0:128], cw[:, a, 0:1], cw[:, a, 4:5], ALU.mult, ALU.add, ["xw", "cw"], [("acc", a)])
                        for k in range(1, 4):
                            STT(acc[:, a, :], xw[:, a, k:k + 128], cw[:, a, k:k + 1], acc[:, a, :], ALU.mult, ALU.add, ["xw", "cw", ("acc", a)], [("acc", a)])
                    ACT(xbT[:], acc[:], AF.Silu, [("acc", a) for a in range(24)], ["xbT"])
                    for half in range(2):
                        p, pk = PS()
                        pb = p[:].bitcast(BF16)
                        for j in range(8):
                            TR(pb[:, j * 128:(j + 1) * 128], xbT[:, half * 8 + j, :], IDB, ["xbT", "CB"], [pk])
                        CP("act" if half == 0 else "dve", xs[:, half * 1024:(half + 1) * 1024], pb[:, 0:1024], [pk], ["xs"])
                    p, pk = PS()
                    pb = p[:].bitcast(BF16)
                    for g in range(4):
                        TR(pb[:, g * 128:(g + 1) * 128], xbT[:, 16 + g, :], IDB, ["xbT", "CB"], [pk])
                    CP("act", Btok[:], pb[:, 0:512], [pk], ["Btok"])
                    TT("dve", dtv[:], dtr[:], dtb[:, dr * 32:(dr + 1) * 32], ALU.add, ["dtr", "dtb"], ["dtv"])
                    ACT(dtv[:], dtv[:], AF.Exp, ["dtv"], ["dtv"])
                    ACT(dtv[:], dtv[:], AF.Ln, ["dtv"], ["dtv"], bias=1.0)
                    TT("dve", av[:], dtv[:], Arow[:, dr * 32:(dr + 1) * 32], ALU.mult, ["dtv", "Arow"], ["av"])
                    p, pk = PS()
                    MM(p[:, 0:32], CO("UF" if dr == 0 else "UB"), av[:], True, True, ["CONST", "av"], [pk])
                    MM(p[:, 32:64], CO("ONES"), av[:], True, True, ["CONST", "av"], [pk])
                    CP("dve", acum[:], p[:, 0:32], [pk], ["acum"])
                    TS("dve", nacum[:], p[:, 0:32], -1.0, None, ALU.mult, None, [pk], ["nacum"])
                    ACT(ea[:], p[:, 0:32], AF.Exp, [pk], ["ea"])
                    ACT(cdv[:], p[:, 32:64], AF.Exp, [pk], ["cdv"])
                    TT("dve", wgt[:], p[:, 32:64], acum[:], ALU.subtract, [pk, "acum"], ["wgt"])
                    ACT(wgt[:], wgt[:], AF.Exp, ["wgt"], ["wgt"])
                    TT("dve", wgt[:], wgt[:], dtv[:], ALU.mult, ["wgt", "dtv"], ["wgt"])
                    TT("dve", xwt[:].rearrange("p (h e) -> p h e", e=64), xs[:].rearrange("p (h e) -> p h e", e=64),
                       wgt[:].unsqueeze(2).to_broadcast([128, 32, 64]), ALU.mult, ["xs", "wgt"], ["xwt"])
                    for g in range(4):
                        p, pk = PS()
                        MM(p[:, 0:128], xbT[:, 16 + g, :], xbT[:, 20 + g, :], True, True, ["xbT"], [pk])
                        CP("act", cbT[:, g, :], p[:, 0:128], [pk], [("cbT", g)])
                    for g in range(4):
                        pyd, pydk = PSX()
                        for hh in range(8):
                            h = g * 8 + hh
                            b = h % 2
                            TS("pool", abc[b][:], CO("ONES"), av[:, h:h + 1], None, ALU.mult, None, ["CONST", "av"], [("abc", b)])
                            pa, pak = PS()
                            MM(pa[:, 0:128], abc[b][:], CO("UF" if dr == 0 else "UB"), True, False, [("abc", b), "CONST"], [pak])
                            MM(pa[:, 0:128], CO("IDF"), CO("NEGF" if dr == 0 else "NEGB"), False, True, ["CONST"], [pak])
                            ACT(Lm[b][:], pa[:, 0:128], AF.Exp, [pak, "nacum"], [("Lm", b)], bias=nacum[:, h:h + 1])
                            STT(WS[b][:], Lm[b][:], dtv[:, h:h + 1], cbT[:, g, :], ALU.mult, ALU.mult, [("Lm", b), "dtv", ("cbT", g)], [("WS", b)])
                            MM(pyd[:, hh * 64:(hh + 1) * 64], WS[b][:], xs[:, h * 64:(h + 1) * 64], True, True, [("WS", b), "xs"], [pydk])
                        CP("act", ydg[:], pyd[:, :], [pydk], ["ydg"])
                        po, pok = PS()
                        MM(po[:, :], xbT[:, 20 + g, :], Ssb[dr][:, g, :], True, True, ["xbT", ("Ssb", dr, g)], [pok])
                        TT("dve", ys[:, g * 512:(g + 1) * 512].rearrange("p (h e) -> p h e", e=64), po[:, :].rearrange("p (h e) -> p h e", e=64),
                           ea[:, g * 8:(g + 1) * 8].unsqueeze(2).to_broadcast([128, 8, 64]), ALU.mult, [pok, "ea"], [("ys", g)])
                        TT("dve", ys[:, g * 512:(g + 1) * 512], ys[:, g * 512:(g + 1) * 512], ydg[:], ALU.add, [("ys", g), "ydg"], [("ys", g)])
                        pst, pstk = PS()
                        MM(pst[:, :], Btok[:, g * 128:(g + 1) * 128], xwt[:, g * 512:(g + 1) * 512], True, True, ["Btok", "xwt"], [pstk])
                        TT("dve", Ss[dr][:, g, :].rearrange("p (h e) -> p h e", e=64), Ss[dr][:, g, :].rearrange("p (h e) -> p h e", e=64),
                           cdv[:, g * 8:(g + 1) * 8].unsqueeze(2).to_broadcast([128, 8, 64]), ALU.mult, [("Ss", dr, g), "cdv"], [("Ss", dr, g)])
                        TT("dve", Ss[dr][:, g, :], Ss[dr][:, g, :], pst[:, :], ALU.add, [("Ss", dr, g), pstk], [("Ss", dr, g)])
                        CP("act", Ssb[dr][:, g, :], Ss[dr][:, g, :], [("Ss", dr, g)], [("Ssb", dr, g)])
                    yrk = [("yr", h) for h in range(8)]
                    ysk = [("ys", g) for g in range(4)]
                    if not last:
                        TT("dve", sq[:].rearrange("p (h e) -> p h e", e=64), xs[:].rearrange("p (h e) -> p h e", e=64),
                           Drow[:].unsqueeze(2).to_broadcast([128, 32, 64]), ALU.mult, ["xs", "Drow"], ["sq"])
                        TT("dve", ys[:], ys[:], sq[:], ALU.add, ysk + ["sq"], ysk)
                        DMA("sp", YRET[t0:t0 + 128, :], yr[:], yrk, ["YRET"])
                        DMA("sp", YSSD[t0:t0 + 128, :], ys[:], ysk, ["YSSD"])
                        return
                    DMA("sp", ypart[:], YRET[t0:t0 + 128, :], ["YRET"], ["ypart"])
                    TT("dve", yr[:], yr[:], ypart[:], ALU.add, yrk + ["ypart"], yrk)
                    RED(st16[:, 0:8], yr[:].rearrange("p (h e) -> p h e", e=256), ALU.add, yrk, ["st16"])
                    ACT(sq[:], yr[:], AF.Square, yrk, ["sq"])
                    RED(st16[:, 8:16], sq[:].rearrange("p (h e) -> p h e", e=256), ALU.add, ["sq"], ["st16"])
                    TS("dve", st16[:, 0:16], st16[:, 0:16], 1.0 / 256, None, ALU.mult, None, ["st16"], ["st16"])
                    TT("dve", st16[:, 16:24], st16[:, 0:8], st16[:, 0:8], ALU.mult, ["st16"], ["st16"])
                    TT("dve", st16[:, 16:24], st16[:, 8:16], st16[:, 16:24], ALU.subtract, ["st16"], ["st16"])
                    TS("dve", st16[:, 16:24], st16[:, 16:24], 0.0, EPS, ALU.max, ALU.add, ["st16"], ["st16"])
                    ACT(st16[:, 16:24], st16[:, 16:24], AF.Ln, ["st16"], ["st16"])
                    ACT(st16[:, 16:24], st16[:, 16:24], AF.Exp, ["st16"], ["st16"], scale=-0.5)
                    TT("dve", st16[:, 24:32], st16[:, 0:8], st16[:, 16:24], ALU.mult, ["st16"], ["st16"])
                    TS("dve", st16[:, 24:32], st16[:, 24:32], -1.0, None, ALU.mult, None, ["st16"], ["st16"])
                    for h in range(8):
                        TS("dve" if h % 2 == 0 else "pool", sq[:, h * 256:(h + 1) * 256], yr[:, h * 256:(h + 1) * 256], st16[:, 16 + h:17 + h], st16[:, 24 + h:25 + h],
                           ALU.mult, ALU.add, yrk + ["st16"], ["sq"])
                    DMA("sp", gz[:], PG[t0:t0 + 128, :], ["PG"], ["gz"])
                    TT("dve", sq[:], sq[:], gng[:], ALU.mult, ["gng", "sq"], ["sq"])
                    TT("dve", ub[:], sq[:], gz[:], ALU.mult, ["sq", "gz"], ["ub"])
                    for half in range(2):
                        p, pk = PS()
                        pb = p[:].bitcast(BF16)
                        for j in range(8):
                            kt = half * 8 + j
                            TR(pb[:, j * 128:(j + 1) * 128], ub[:, kt * 128:(kt + 1) * 128], IDB, ["ub", "CB"], [pk])
                        CP("act", uT[:, half * 8:(half + 1) * 8, :], pb[:, 0:1024].rearrange("p (k t) -> p k t", t=128), [pk], ["uT"])
                    DMA("sp", UT[0, :, t0:t0 + 128].rearrange("(k p) t -> p k t", p=128), uT[:], ["uT"], ["UT"])
                    DMA("sp", ypart[:], YSSD[t0:t0 + 128, :], ["YSSD"], ["ypart"])
                    TT("dve", ys[:], ys[:], ypart[:], ALU.add, ysk + ["ypart"], ysk)
                    DMA("sp", gz[:], PZ[t0:t0 + 128, :], ["PZ"], ["gz"])
                    TT("dve", ys[:], ys[:], gz[:], ALU.mult, ysk + ["gz"], ysk)
                    ACT(sq[:], ys[:], AF.Square, ysk, ["sq", "st16b"], accum_out=st16[:, 32:33])
                    ACT(st16[:, 33:34], st16[:, 32:33], AF.Ln, ["st16b"], ["st16b"], bias=EPS, scale=1.0 / D)
                    ACT(st16[:, 34:35], st16[:, 33:34], AF.Exp, ["st16b"], ["st16b"], scale=-0.5)
                    STT(ub[:], ys[:], st16[:, 34:35], sng[:], ALU.mult, ALU.mult, ysk + ["st16b", "sng"], ["ub"])
                    for half in range(2):
                        p, pk = PS()
                        pb = p[:].bitcast(BF16)
                        for j in range(8):
                            kt = half * 8 + j
                            TR(pb[:, j * 128:(j + 1) * 128], ub[:, kt * 128:(kt + 1) * 128], IDB, ["ub", "CB"], [pk])
                        CP("act", uT[:, half * 8:(half + 1) * 8, :], pb[:, 0:1024].rearrange("p (k t) -> p k t", t=128), [pk], ["uT"])
                    DMA("sp", UT[1, :, t0:t0 + 128].rearrange("(k p) t -> p k t", p=128), uT[:], ["uT"], ["UT"])

                for (s0, sn) in seqs:
                    latent = s0 != 0
                    nch = sn // 128
                    for dr in range(2):
                        order = range(nch) if dr == 0 else range(nch - 1, -1, -1)
                        for ci in order:
                            if latent and os.environ.get("MK_CHUNKS"):
                                lo, hi = [int(v) for v in os.environ["MK_CHUNKS"].split(",")]
                                if not (lo <= ci < hi):
                                    continue
                            chunk(s0 + ci * 128, latent, dr, dr == 1)
                            S.maybe_flush()
                S.flush()
            S.barrier()

        def phase_LRU(li):
            with contextlib.ExitStack() as st:
                def sb(name, shape, dt):
                    return st.enter_context(nc.sbuf_tensor(U(name), list(shape), dt))
                NB = 512
                GW = sb("GW", [128, 64, 128], BF16)
                LR = sb("LR", [11, D], F32)
                LP = sb("LP", [128, 16, 11], F32)
                carry = sb("carry", [128, 32], F32)
                rw_ = sb("recw", [128, 16, NB + 3], F32)
                xr = sb("xr", [128, 16, NB], F32)
                xrb = sb("xrb", [128, 16, NB], BF16)
                rg = [sb("rg%d" % i, [128, NB], F32) for i in range(2)]
                ig = [sb("ig%d" % i, [128, NB], F32) for i in range(2)]
                aa = [sb("aa%d" % i, [128, NB], F32) for i in range(2)]
                mm_ = [sb("mmm%d" % i, [128, NB], F32) for i in range(2)]
                bx = [sb("bx%d" % i, [128, NB], F32) for i in range(2)]
                hh = [sb("hh%d" % i, [128, NB], F32) for i in range(2)]
                hf = [sb("hfl%d" % i, [128, NB], F32) for i in range(2)]
                gl = [sb("gl%d" % i, [128, NB], BF16) for i in range(2)]
                uo = [sb("uo%d" % i, [128, NB], BF16) for i in range(2)]
                DMA("pool", GW[:], lru_gate_w[li, :, :, :].rearrange("a d e -> d a e"), [], ["GW"])
                DMA("sp", LR[0:4, :], lru_conv_w[li, :, :], [], ["LR"])
                DMA("sp", LR[4:5, :], lru_conv_b[li:li + 1, :], [], ["LR"])
                DMA("sp", LR[5:7, :], lru_lambda[li, :, :], [], ["LR"])
                DMA("sp", LR[7:11, :], lru_gate_b[li, :, :], [], ["LR"])
                p, pk = PS()
                for a in range(16):
                    TR(p[:, a * 11:(a + 1) * 11], LR[0:11, a * 128:(a + 1) * 128], CO("IDF")[0:11, 0:11], ["LR", "CONST"], [pk])
                CP("dve", LP[:].rearrange("p a k -> p (a k)"), p[:, 0:176], [pk], ["LP"])
                ACT(LP[:, :, 5:7], LP[:, :, 5:7], AF.Exp, ["LP"], ["LP"], scale=-1.0)
                ACT(LP[:, :, 5:7], LP[:, :, 5:7], AF.Ln, ["LP"], ["LP"], bias=1.0)
                TS("dve", LP[:, :, 5:7], LP[:, :, 5:7], -8.0, None, ALU.mult, None, ["LP"], ["LP"])
                for (s0, sn) in seqs:
                    blocks = []
                    o = 0
                    while o < sn:
                        n = min(NB, sn - o)
                        blocks.append((s0 + o, n))
                        o += n
                    if s0 == 0:
                        MEMSET("pool", carry[:], 0.0, ["carry"])
                    for dr in range(2):
                        blks = blocks if dr == 0 else blocks[::-1]
                        for (t0, n) in blks:
                            S.maybe_flush()
                            c0 = colof(t0)
                            DMA("sp", rw_[:, :, 0:n + 3], PREC[:, :, c0 - 2:c0 + n + 1], ["PREC"], ["recw"])
                            for a in range(16):
                                eng = "dve" if a % 2 == 0 else "pool"
                                TS(eng, xr[:, a, 0:n], rw_[:, a, 0:n], LP[:, a, 0:1], LP[:, a, 4:5], ALU.mult, ALU.add, ["recw", "LP"], [("xr", a)])
                                for k in range(1, 4):
                                    STT(xr[:, a, 0:n], rw_[:, a, k:k + n], LP[:, a, k:k + 1], xr[:, a, 0:n], ALU.mult, ALU.add, ["recw", "LP", ("xr", a)], [("xr", a)])
                                CP("act", xrb[:, a, 0:n], xr[:, a, 0:n], [("xr", a)], [("xrb", a)])
                            for a in range(16):
                                b = a % 2
                                ci = dr * 16 + a
                                pr, prk = PS()
                                MM(pr[:, 0:n], GW[:, (dr * 2 + 0) * 16 + a, :], xrb[:, a, 0:n], True, True, ["GW", ("xrb", a)], [prk])
                                pi, pik = PS()
                                MM(pi[:, 0:n], GW[:, (dr * 2 + 1) * 16 + a, :], xrb[:, a, 0:n], True, True, ["GW", ("xrb", a)], [pik])
                                ACT(rg[b][:, 0:n], pr[:, 0:n], AF.Sigmoid, [prk, "LP"], [("rg", b)], bias=LP[:, a, 7 + dr * 2:8 + dr * 2])
                                ACT(ig[b][:, 0:n], pi[:, 0:n], AF.Sigmoid, [pik, "LP"], [("ig", b)], bias=LP[:, a, 8 + dr * 2:9 + dr * 2])
                                ACT(aa[b][:, 0:n], rg[b][:, 0:n], AF.Exp, [("rg", b), "LP"], [("aa", b)], scale=LP[:, a, 5 + dr:6 + dr])
                                TT("pool", mm_[b][:, 0:n], aa[b][:, 0:n], aa[b][:, 0:n], ALU.mult, [("aa", b)], [("mm", b)])
                                TS("pool", mm_[b][:, 0:n], mm_[b][:, 0:n], -1.0, 1.0, ALU.mult, ALU.add, [("mm", b)], [("mm", b)])
                                TS("pool", mm_[b][:, 0:n], mm_[b][:, 0:n], 1e-20, None, ALU.max, None, [("mm", b)], [("mm", b)])
                                ACT(mm_[b][:, 0:n], mm_[b][:, 0:n], AF.Sqrt, [("mm", b)], [("mm", b)])
                                TT("pool", bx[b][:, 0:n], ig[b][:, 0:n], xr[:, a, 0:n], ALU.mult, [("ig", b), ("xr", a)], [("bx", b)])
                                TT("dve", bx[b][:, 0:n], bx[b][:, 0:n], mm_[b][:, 0:n], ALU.mult, [("bx", b), ("mm", b)], [("bx", b)])
                                if dr == 0:
                                    S.op("dve", lambda e, b=b, n=n, ci=ci: e.tensor_tensor_scan(out=hh[b][:, 0:n], data0=aa[b][:, 0:n], data1=bx[b][:, 0:n],
                                         initial=carry[:, ci:ci + 1], op0=ALU.mult, op1=ALU.add), [("aa", b), ("bx", b), "carry"], [("hh", b)])
                                    CP("dve", carry[:, ci:ci + 1], hh[b][:, n - 1:n], [("hh", b)], ["carry"])
                                    DMA("sp", HF[a * 128:(a + 1) * 128, t0:t0 + n], hh[b][:, 0:n], [("hh", b)], ["HF"])
                                else:
                                    S.op("dve", lambda e, b=b, n=n, ci=ci: e.tensor_tensor_scan(out=hh[b][:, 0:n][:, ::-1],
                                         data0=aa[b][:, 0:n][:, ::-1], data1=bx[b][:, 0:n][:, ::-1],
                                         initial=carry[:, ci:ci + 1], op0=ALU.mult, op1=ALU.add), [("aa", b), ("bx", b), "carry"], [("hh", b)])
                                    CP("dve", carry[:, ci:ci + 1], hh[b][:, 0:1], [("hh", b)], ["carry"])
                                    DMA("sp", hf[b][:, 0:n], HF[a * 128:(a + 1) * 128, t0:t0 + n], ["HF"], [("hf", b)])
                                    DMA("sp", gl[b][:, 0:n], PGG[a * 128:(a + 1) * 128, t0:t0 + n], ["PGG"], [("gl", b)])
                                    TT("pool", hh[b][:, 0:n], hh[b][:, 0:n], hf[b][:, 0:n], ALU.add, [("hh", b), ("hf", b)], [("hh", b)])
                                    TT("pool", uo[b][:, 0:n], hh[b][:, 0:n], gl[b][:, 0:n], ALU.mult, [("hh", b), ("gl", b)], [("uo", b)])
                                    DMA("sp", UT[2, a * 128:(a + 1) * 128, t0:t0 + n], uo[b][:, 0:n], [("uo", b)], ["UT"])
                S.flush()
            S.barrier()

        def residual_tm(actT, akey, KTn, nblk, W_ap_fn, wbufs, wkname, grow, t0, n, xt, yk):
            nsub = n // 128
            for cb in range(4):
                wt = wbufs[cb % 2]; wk = (wkname, cb % 2)
                DMA("pool", wt[:], W_ap_fn(cb), [], [wk])
                for s in range(nsub):
                    p, pk = PS()
                    for kt in range(KTn):
                        MM(p[:, :], actT[:, kt, s * 128:(s + 1) * 128], wt[:, kt, :], kt == 0, kt == KTn - 1, [akey, wk], [pk])
                    TT("dve", p[:, :], p[:, :], grow[:, cb * 512:(cb + 1) * 512], ALU.mult, [pk, "grow"], [pk])
                    TT("dve", xt[s][:, cb * 512:(cb + 1) * 512], xt[s][:, cb * 512:(cb + 1) * 512], p[:, :], ALU.add, [pk, ("xres", s)], [("xres", s)])

        def phase_C(li, skip_ctx):
            with contextlib.ExitStack() as st:
                def sb(name, shape, dt):
                    return st.enter_context(nc.sbuf_tensor(U(name), list(shape), dt))
                Ub = [sb("Ub%d" % k, [128, KT, 512], BF16) for k in range(3)]
                wb = [sb("wbr%d" % i, [128, KT, 128], BF16) for i in range(6)]
                sg = [sb("sgt%d" % i, [128, 512], BF16) for i in range(3)]
                mT = sb("mT", [128, KT, 512], BF16)
                t32 = sb("t32", [128, 512], F32); t32b = sb("t32b", [128, 512], F32)
                wo = [sb("wo%d" % i, [128, KT, 512], BF16) for i in range(2)]
                grow = sb("grow", [128, D], F32)
                xt = [sb("xres%d" % i, [128, D], F32) for i in range(4)]
                wi = [0]
                for (t0, n) in tok_blocks(skip_ctx):
                    S.maybe_flush()
                    row = 1 if t0 < Tc else 0
                    DMA("sp", grow[:], MODD[li, row:row + 1, 2 * D:3 * D].partition_broadcast(128), [("MODD", li)], ["grow"])
                    for k in range(3):
                        DMA("sp", Ub[k][:, :, 0:n], UT[k, :, t0:t0 + n].rearrange("(kt p) t -> p kt t", p=128), ["UT"], [("Ub", k)])
                    for s in range(n // 128):
                        DMA("sp", xt[s][:], X[t0 + s * 128:t0 + (s + 1) * 128, :], ["X"], [("xres", s)])
                    for dt_ in range(KT):
                        pks = []
                        for k in range(3):
                            w = wb[wi[0] % 6]; wk = ("wbr", wi[0] % 6); wi[0] += 1
                            DMA("pool", w[:], w_branch[li][k][:, dt_ * 128:(dt_ + 1) * 128].rearrange("(kt p) c -> p kt c", p=128), [], [wk])
                            DMA("sp", sg[k][:, 0:n], PSG[k * 2048 + dt_ * 128:k * 2048 + (dt_ + 1) * 128, t0:t0 + n], ["PSG"], [("sg", k)])
                            p, pk = PS()
                            for kt in range(KT):
                                MM(p[:, 0:n], w[:, kt, :], Ub[k][:, kt, 0:n], kt == 0, kt == KT - 1, [wk, ("Ub", k)], [pk])
                            pks.append((p, pk))
                        TT("dve", t32[:, 0:n], pks[0][0][:, 0:n], sg[0][:, 0:n], ALU.mult, [pks[0][1], ("sg", 0)], ["t32"])
                        TT("dve", t32b[:, 0:n], pks[1][0][:, 0:n], sg[1][:, 0:n], ALU.mult, [pks[1][1], ("sg", 1)], ["t32b"])
                        TT("pool", t32[:, 0:n], t32[:, 0:n], t32b[:, 0:n], ALU.add, ["t32", "t32b"], ["t32"])
                        TT("dve", t32b[:, 0:n], pks[2][0][:, 0:n], sg[2][:, 0:n], ALU.mult, [pks[2][1], ("sg", 2)], ["t32b"])
                        TT("pool", mT[:, dt_, 0:n], t32[:, 0:n], t32b[:, 0:n], ALU.add, ["t32", "t32b"], ["mT"])
                    residual_tm(mT, "mT", KT, 4, lambda cb: w_out[li][:, cb * 512:(cb + 1) * 512].rearrange("(kt p) c -> p kt c", p=128),
                                wo, "wo", grow, t0, n, xt, None)
                    for s in range(n // 128):
                        DMA("sp", X[t0 + s * 128:t0 + (s + 1) * 128, :], xt[s][:], [("xres", s)], ["X"])
                S.flush()
            S.barrier()

        def phase_F1(HT, w1_fn, w3_fn, nff_tiles, skip_ctx, gate_of=None):
            with contextlib.ExitStack() as st:
                def sb(name, shape, dt):
                    return st.enter_context(nc.sbuf_tensor(U(name), list(shape), dt))
                w1b = [sb("w1b%d" % i, [128, KT, 128], BF16) for i in range(2)]
                w3b = [sb("w3b%d" % i, [128, KT, 128], BF16) for i in range(2)]
                s1 = [sb("s1_%d" % i, [128, 512], F32) for i in range(2)]
                ho = [sb("ho%d" % i, [128, 512], BF16) for i in range(2)]
                gate = None
                if gate_of is not None:
                    gate = sb("gateb", [128, T], F32)
                blks = tok_blocks(skip_ctx)
                cnt = 0
                cur_e = -1
                for ft in range(nff_tiles):
                    S.maybe_flush()
                    b = ft % 2
                    DMA("pool", w1b[b][:], w1_fn(ft), [], [("w1b", b)])
                    DMA("pool", w3b[b][:], w3_fn(ft), [], [("w3b", b)])
                    if gate_of is not None and gate_of(ft) != cur_e:
                        cur_e = gate_of(ft)
                        DMA("sp", gate[:], GATET[cur_e:cur_e + 1, :].partition_broadcast(128), ["GATET"], ["gate"])
                    for (t0, n) in blks:
                        c0 = colof(t0)
                        p1, p1k = PS()
                        for kt in range(KT):
                            MM(p1[:, 0:n], w1b[b][:, kt, :], HT[:, kt, c0:c0 + n], kt == 0, kt == KT - 1, [("w1b", b), "HTall"], [p1k])
                        p3, p3k = PS()
                        for kt in range(KT):
                            MM(p3[:, 0:n], w3b[b][:, kt, :], HT[:, kt, c0:c0 + n], kt == 0, kt == KT - 1, [("w3b", b), "HTall"], [p3k])
                        i = cnt % 2; cnt += 1
                        ACT(s1[i][:, 0:n], p1[:, 0:n], AF.Silu, [p1k], [("s1", i)])
                        if gate is None:
                            TT("dve", ho[i][:, 0:n], s1[i][:, 0:n], p3[:, 0:n], ALU.mult, [("s1", i), p3k], [("ho", i)])
                        else:
                            TT("dve", s1[i][:, 0:n], s1[i][:, 0:n], p3[:, 0:n], ALU.mult, [("s1", i), p3k], [("s1", i)])
                            TT("pool", ho[i][:, 0:n], s1[i][:, 0:n], gate[:, t0:t0 + n], ALU.mult, [("s1", i), "gate"], [("ho", i)])
                        DMA("sp", AHv(ft * 128, 128, t0, n), ho[i][:, 0:n], [("ho", i)], ["AH"])
                S.flush()
            S.barrier()

        def phase_F2(li, w2_fn, nkt, kc, skip_ctx):
            with contextlib.ExitStack() as st:
                def sb(name, shape, dt):
                    return st.enter_context(nc.sbuf_tensor(U(name), list(shape), dt))
                hb_ = [sb("hck%d" % i, [128, kc, 512], BF16) for i in range(2)]
                wb_ = [sb("w2c%d" % i, [128, kc, 512], BF16) for i in range(2)]
                grow = sb("grow2", [128, D], F32)
                xt = [sb("xres%d" % i, [128, D], F32) for i in range(4)]
                nkc = nkt // kc
                hi = 0; wi = 0
                for (t0, n) in tok_blocks(skip_ctx):
                    S.maybe_flush()
                    nsub = n // 128
                    row = 1 if t0 < Tc else 0
                    DMA("sp", grow[:], MODD[li, row:row + 1, 5 * D:6 * D].partition_broadcast(128), [("MODD", li)], ["grow"])
                    for s in range(nsub):
                        DMA("sp", xt[s][:], X[t0 + s * 128:t0 + (s + 1) * 128, :], ["X"], [("xres", s)])
                    for cb2 in range(2):
                        pss = [[(psb[c2_ * 4 + s], ("ps", c2_ * 4 + s)) for s in range(nsub)] for c2_ in range(2)]
                        for kci in range(nkc):
                            S.maybe_flush()
                            h_ = hb_[hi % 2]; hk = ("hck", hi % 2); hi += 1
                            DMA("sp", h_[:, :, 0:n], AHv(kci * kc * 128, kc * 128, t0, n).rearrange("(k p) t -> p k t", p=128), ["AH"], [hk])
                            for c2 in range(2):
                                cb = cb2 * 2 + c2
                                w_ = wb_[wi % 2]; wk = ("w2c", wi % 2); wi += 1
                                DMA("pool", w_[:], w2_fn(kci * kc, kc, cb), [], [wk])
                                for s in range(nsub):
                                    p, pk = pss[c2][s]
                                    for k in range(kc):
                                        MM(p[:, :], h_[:, k, s * 128:(s + 1) * 128], w_[:, k, :], kci == 0 and k == 0, kci == nkc - 1 and k == kc - 1, [hk, wk], [pk])
                        for c2 in range(2):
                            cb = cb2 * 2 + c2
                            for s in range(nsub):
                                p, pk = pss[c2][s]
                                TT("dve", p[:, :], p[:, :], grow[:, cb * 512:(cb + 1) * 512], ALU.mult, [pk, "grow"], [pk])
                                TT("dve", xt[s][:, cb * 512:(cb + 1) * 512], xt[s][:, cb * 512:(cb + 1) * 512], p[:, :], ALU.add, [pk, ("xres", s)], [("xres", s)])
                    for s in range(nsub):
                        DMA("sp", X[t0 + s * 128:t0 + (s + 1) * 128, :], xt[s][:], [("xres", s)], ["X"])
                S.flush()
            S.barrier()

        def phase_final():
            with contextlib.ExitStack() as st:
                def sb(name, shape, dt):
                    return st.enter_context(nc.sbuf_tensor(U(name), list(shape), dt))
                gg = sb("fgg", [128, D], F32)
                xt = [sb("fx%d" % i, [128, D], F32) for i in range(2)]
                ot = [sb("fo%d" % i, [128, D], F32) for i in range(2)]
                tmp = sb("ftmp", [128, D], F32)
                st8 = sb("fst8", [128, 8], F32)
                DMA("sp", gg[:], final_g[0:1, :].partition_broadcast(128), [], ["gg"])
                for ti in range(Tl // 128):
                    b = ti % 2
                    t0 = Tc + ti * 128
                    DMA("sp", xt[b][:], X[t0:t0 + 128, :], ["X"], [("fx", b)])
                    ACT(tmp[:], xt[b][:], AF.Square, [("fx", b)], ["ftmp", "fst8"], accum_out=st8[:, 0:1])
                    ACT(st8[:, 1:2], st8[:, 0:1], AF.Ln, ["fst8"], ["fst8"], bias=EPS, scale=1.0 / D)
                    ACT(st8[:, 2:3], st8[:, 1:2], AF.Exp, ["fst8"], ["fst8"], scale=-0.5)
                    STT(ot[b][:], xt[b][:], st8[:, 2:3], gg[:], ALU.mult, ALU.mult, [("fx", b), "fst8", "gg"], [("fo", b)])
                    DMA("sp", out[ti * 128:(ti + 1) * 128, :], ot[b][:], [("fo", b)], ["out"])
                S.flush(final=True)

        _pc = [0]
        _stop = int(os.environ.get('MK_STOP', '0'))

        def go():
            _pc[0] += 1
            return _stop == 0 or _pc[0] <= _stop

        for li in range(depth if nlayers is None else nlayers):
            last = li == depth - 1
            is_moe = (li % 2 == 1)
            j = li // 2
            with contextlib.ExitStack() as hst:
                HT = hst.enter_context(nc.sbuf_tensor("HT_a%d" % li, [128, KT, max(TP, int(os.environ.get("MK_HTPAD", "0")))], BF16))
                if go(): phase_A(HT, li, 0, norm1_g[li:li + 1, :], False)
                if go(): phase_P(HT, li)
            if go(): phase_MIX(li)
            if go(): phase_LRU(li)
            if go(): phase_C(li, last)
            with contextlib.ExitStack() as hst:
                HT = hst.enter_context(nc.sbuf_tensor("HT_b%d" % li, [128, KT, max(TP, int(os.environ.get("MK_HTPAD", "0")))], BF16))
                if not is_moe:
                    if go(): phase_A(HT, li, 1, norm2_g[li:li + 1, :], last)
                    nff = dff // 128
                    if go(): phase_F1(HT, lambda ft: ffn_w1[j][:, ft * 128:(ft + 1) * 128].rearrange("(kt p) c -> p kt c", p=128),
                             lambda ft: ffn_w3[j][:, ft * 128:(ft + 1) * 128].rearrange("(kt p) c -> p kt c", p=128), nff, last)
                else:
                    if go(): phase_A(HT, li, 1, norm2_g[li:li + 1, :], last, router=(router_w[j, :, :], router_b[j:j + 1, :]))
                    tpe = dffe // 128
                    if go(): phase_F1(HT, lambda ft: moe_w1[j][ft // tpe][:, (ft % tpe) * 128:(ft % tpe + 1) * 128].rearrange("(kt p) c -> p kt c", p=128),
                             lambda ft: moe_w3[j][ft // tpe][:, (ft % tpe) * 128:(ft % tpe + 1) * 128].rearrange("(kt p) c -> p kt c", p=128),
                             nexp * tpe, last, gate_of=lambda ft: ft // tpe)
            if not is_moe:
                nkt = dff // 128
                kc = nkt if nkt <= 32 else max(k for k in (1, 2, 4, 8, 16, 32) if nkt % k == 0)
                if go(): phase_F2(li, lambda k0, kn, cb: ffn_w2[j][k0 * 128:(k0 + kn) * 128, cb * 512:(cb + 1) * 512].rearrange("(k p) c -> p k c", p=128), nkt, kc, last)
            else:
                nkt = nexp * dffe // 128
                kcm = min(32, dffe // 128)
                tpe2 = dffe // 128
                if go(): phase_F2(li, lambda k0, kn, cb: moe_w2[j][k0 // tpe2][(k0 % tpe2) * 128:(k0 % tpe2 + kn) * 128, cb * 512:(cb + 1) * 512].rearrange("(k p) c -> p k c", p=128), nkt, kcm, last)
        phase_final()
    return nc, S


def make_in_maps(inputs, Tc, Tl, depth, nb):
    carr, coff, rot = make_consts(Tl)
    f = lambda a: np.ascontiguousarray(np.asarray(a, dtype=np.float32))
    maps = []
    for b in range(nb):
        m = {}
        m["x"] = f(inputs["x"][b]); m["ctx"] = f(inputs["ctx"][b])
        m["c"] = f(inputs["c"][b:b + 1]); m["c_ctx"] = f(np.asarray(inputs["c_ctx"])[None, :])
        m["b_mod"] = f(inputs["b_mod"])
        m["norm1_g"] = f(inputs["norm1_g"]); m["norm2_g"] = f(inputs["norm2_g"])
        for l in range(depth):
            m["w_mod%d" % l] = f(inputs["w_mod"][l]); m["w_in%d" % l] = f(inputs["w_in"][l]); m["w_out%d" % l] = f(inputs["w_out"][l])
            for k in range(3):
                m["w_branch%d_%d" % (l, k)] = f(inputs["w_branch"][l][k])
        for l in range(np.asarray(inputs["ffn_w1"]).shape[0]):
            m["ffn_w1_%d" % l] = f(inputs["ffn_w1"][l]); m["ffn_w3_%d" % l] = f(inputs["ffn_w3"][l]); m["ffn_w2_%d" % l] = f(inputs["ffn_w2"][l])
        m["ret_decay"] = f(np.asarray(inputs["ret_decay"]).reshape(depth, 16)); m["ret_gn_g"] = f(inputs["ret_gn_g"])
        m["ssd_conv_w"] = f(inputs["ssd_conv_w"]); m["ssd_conv_b"] = f(inputs["ssd_conv_b"])
        m["ssd_dt_bias"] = f(np.asarray(inputs["ssd_dt_bias"]).reshape(depth, 64))
        m["ssd_a_log"] = f(np.asarray(inputs["ssd_a_log"]).reshape(depth, 64))
        m["ssd_d"] = f(inputs["ssd_d"]); m["ssd_norm_g"] = f(inputs["ssd_norm_g"])
        m["lru_conv_w"] = f(inputs["lru_conv_w"]); m["lru_conv_b"] = f(inputs["lru_conv_b"])
        m["lru_gate_w"] = f(np.asarray(inputs["lru_gate_w"]).reshape(depth, 64, 128, 128))
        m["lru_gate_b"] = f(np.asarray(inputs["lru_gate_b"]).reshape(depth, 4, D))
        m["lru_lambda"] = f(inputs["lru_lambda"])
        if depth >= 2:
            m["router_w"] = f(inputs["router_w"]); m["router_b"] = f(inputs["router_b"])
            for l in range(depth // 2):
                for e in range(np.asarray(inputs["moe_w1"]).shape[1]):
                    m["moe_w1_%d_%d" % (l, e)] = f(inputs["moe_w1"][l][e]); m["moe_w3_%d_%d" % (l, e)] = f(inputs["moe_w3"][l][e])
                    m["moe_w2_%d_%d" % (l, e)] = f(inputs["moe_w2"][l][e])
        m["final_g"] = f(np.asarray(inputs["final_g"])[None, :])
        m["consts"] = carr; m["rot"] = rot
        maps.append(m)
    return maps


def kernel(**inputs):
    x = np.asarray(inputs["x"])
    nb, Tl, _ = x.shape
    Tc = np.asarray(inputs["ctx"]).shape[1]
    depth = np.asarray(inputs["w_in"]).shape[0]
    n_dense = np.asarray(inputs["ffn_w1"]).shape[0]
    n_moe = np.asarray(inputs["moe_w1"]).shape[0] if depth >= 2 else 0
    import os
    nc, _ = build(Tc, Tl, depth, n_dense, n_moe, debug=bool(os.environ.get('MK_DEBUG')), nlayers=(int(os.environ['MK_NLAYERS']) if os.environ.get('MK_NLAYERS') else None),
                  dff=np.asarray(inputs["ffn_w1"]).shape[2],
                  nexp=np.asarray(inputs["moe_w1"]).shape[1], dffe=np.asarray(inputs["moe_w1"]).shape[3])
    maps = make_in_maps(inputs, Tc, Tl, depth, nb)
    res = run_bass_kernel_spmd(nc, maps, core_ids=list(range(nb)))
    return np.stack([res.results[b]["out"] for b in range(nb)], axis=0).astype(np.float32)
```
